# Optimizing a Trainium2 kernel written in Bass

```python
import math
import numpy as np
import jax
import jax.numpy as jnp
from jax import lax

D_MODEL = 1024
BATCH = 4
SEQ = 4096
DEPTH = 2

HEAD_DIM = 64
N_MIXERS = 4
GROUP_WIDTH = D_MODEL // N_MIXERS
N_HEADS = GROUP_WIDTH // HEAD_DIM
ROPE_THETA = 10000.0
EPS = 1e-6
Q_BLOCK = 128
NEG_INF = -1e30

NSA_CMP_LEN = 32
NSA_CMP_STRIDE = 16
NSA_SLC_LEN = 64
NSA_TOP_N = 16
NSA_WINDOW = 512
NSA_FORCED_LOCAL = 2

DIFF_HALF = HEAD_DIM // 2

MLSTM_CHUNK = 64
MLSTM_CONV = 4

DILATED_PATTERNS = ((128, 1), (512, 4), (2048, 16))
DILATED_PAD = 2048

D_FF = ((8 * D_MODEL + 3 * 256 - 1) // (3 * 256)) * 256

IN_SPLITS = (
    GROUP_WIDTH,
    HEAD_DIM, HEAD_DIM,
    HEAD_DIM, HEAD_DIM,
    HEAD_DIM, HEAD_DIM,
    3 * N_HEADS,
    GROUP_WIDTH, GROUP_WIDTH, GROUP_WIDTH,
    GROUP_WIDTH, GROUP_WIDTH,
    N_HEADS, N_HEADS, GROUP_WIDTH,
    GROUP_WIDTH, GROUP_WIDTH, GROUP_WIDTH,
)
D_IN = sum(IN_SPLITS)

kernel_name = 'hybrid_nsa_diff_mlstm_dilated_trunk'


def rmsnorm(x, g):
    xf = x.astype(jnp.float32)
    y = xf * lax.rsqrt(jnp.mean(xf * xf, axis=-1, keepdims=True) + EPS)
    return (y * g.astype(jnp.float32)).astype(x.dtype)


def rope_tables(seq, dim):
    inv = 1.0 / (ROPE_THETA ** (jnp.arange(0, dim, 2, dtype=jnp.float32) / dim))
    ang = jnp.arange(seq, dtype=jnp.float32)[:, None] * inv[None, :]
    return jnp.cos(ang), jnp.sin(ang)


def apply_rope(x, cos, sin):
    shape = (cos.shape[0],) + (1,) * (x.ndim - 3) + (cos.shape[1],)
    c, s = cos.reshape(shape), sin.reshape(shape)
    xf = x.astype(jnp.float32)
    half = x.shape[-1] // 2
    x1, x2 = xf[..., :half], xf[..., half:]
    return jnp.concatenate([x1 * c - x2 * s, x1 * s + x2 * c], axis=-1).astype(x.dtype)


def masked_softmax(s, mask, return_lse=False):
    s = jnp.where(mask, s.astype(jnp.float32), NEG_INF)
    m = jnp.max(s, axis=-1, keepdims=True)
    e = jnp.where(mask, jnp.exp(s - m), 0.0)
    z = jnp.sum(e, axis=-1, keepdims=True)
    z_safe = jnp.where(z > 0, z, 1.0)
    p = e / z_safe
    if return_lse:
        return p, (m + jnp.log(z_safe))[..., 0]
    return p


def sweep_query_blocks(fn, seq):
    out = lax.map(fn, jnp.arange(seq // Q_BLOCK) * Q_BLOCK)
    out = jnp.moveaxis(out, 0, 1)
    return out.reshape((out.shape[0], seq) + out.shape[3:])


def nsa_mixer(q, k_cmp, v_cmp, k_slc, v_slc, k_win, v_win, gate_pre, cmp_pos, cmp_w, cos, sin):
    bsz, seq = q.shape[0], q.shape[1]
    scale = HEAD_DIM ** -0.5
    t_pos = jnp.arange(seq)

    n_cmp = (seq - NSA_CMP_LEN) // NSA_CMP_STRIDE + 1
    blk_idx = np.arange(n_cmp)[:, None] * NSA_CMP_STRIDE + np.arange(NSA_CMP_LEN)[None, :]

    def compress(t, pos_emb, w):
        blocks = t[:, blk_idx] + pos_emb
        return blocks.reshape(bsz, n_cmp, NSA_CMP_LEN * HEAD_DIM) @ w

    kc = compress(k_cmp, cmp_pos[0], cmp_w[0])
    vc = compress(v_cmp, cmp_pos[1], cmp_w[1])
    cmp_end = jnp.arange(n_cmp) * NSA_CMP_STRIDE + NSA_CMP_LEN - 1
    cmp_mask = cmp_end[None, :] <= t_pos[:, None]
    p_cmp = masked_softmax(jnp.einsum('bshd,bnd->bhsn', q, kc) * scale, cmp_mask)
    o_cmp = jnp.einsum('bhsn,bnd->bshd', p_cmp.astype(vc.dtype), vc)

    n_slc = seq // NSA_SLC_LEN
    top_n = min(NSA_TOP_N, n_slc)
    ratio_s, ratio_c = NSA_SLC_LEN // NSA_CMP_STRIDE, NSA_CMP_LEN // NSA_CMP_STRIDE
    jj = np.arange(n_slc)[:, None, None]
    src = ratio_s * jj - np.arange(ratio_s)[None, :, None] - np.arange(ratio_c)[None, None, :]
    ok = (src >= 0) & (src < n_cmp)
    cmp_to_slc = np.zeros((n_cmp, n_slc), np.float32)
    np.add.at(cmp_to_slc, (np.where(ok, src, 0), np.broadcast_to(jj, src.shape)), ok.astype(np.float32))
    importance = jnp.einsum('bhsn,nj->bsj', p_cmp, jnp.asarray(cmp_to_slc))
    blk = jnp.arange(n_slc)[None, :]
    cur = (t_pos // NSA_SLC_LEN)[:, None]
    forced = (blk == 0) | ((blk <= cur) & (blk > cur - NSA_FORCED_LOCAL))
    score = jnp.where(blk > cur, -1.0e6, jnp.where(forced, 1.0e6, importance))
    _, sel_idx = lax.top_k(score, top_n)
    sel_valid = sel_idx <= cur[None]

    q_rot = apply_rope(q, cos, sin)
    ks = apply_rope(k_slc, cos, sin).reshape(bsz, n_slc, NSA_SLC_LEN, HEAD_DIM)
    vs = v_slc.reshape(bsz, n_slc, NSA_SLC_LEN, HEAD_DIM)
    pad = ((0, 0), (NSA_WINDOW, 0), (0, 0))
    kw = jnp.pad(apply_rope(k_win, cos, sin), pad)
    vw = jnp.pad(v_win, pad)
    b_idx = jnp.arange(bsz)[:, None, None]
    n_sel_keys = top_n * NSA_SLC_LEN

    def block(s0):
        qb = lax.dynamic_slice_in_dim(q_rot, s0, Q_BLOCK, axis=1)
        tq = s0 + jnp.arange(Q_BLOCK)
        idx = lax.dynamic_slice_in_dim(sel_idx, s0, Q_BLOCK, axis=1)
        valid = lax.dynamic_slice_in_dim(sel_valid, s0, Q_BLOCK, axis=1)
        kg = ks[b_idx, idx].reshape(bsz, Q_BLOCK, n_sel_keys, HEAD_DIM)
        vg = vs[b_idx, idx].reshape(bsz, Q_BLOCK, n_sel_keys, HEAD_DIM)
        kpos = (idx[..., None] * NSA_SLC_LEN + jnp.arange(NSA_SLC_LEN)).reshape(bsz, Q_BLOCK, n_sel_keys)
        kvalid = jnp.repeat(valid, NSA_SLC_LEN, axis=-1) & (kpos <= tq[None, :, None])
        p = masked_softmax(jnp.einsum('bqhd,bqkd->bhqk', qb, kg) * scale, kvalid[:, None])
        o_slc = jnp.einsum('bhqk,bqkd->bqhd', p.astype(vg.dtype), vg)
        kwb = lax.dynamic_slice_in_dim(kw, s0, NSA_WINDOW + Q_BLOCK, axis=1)
        vwb = lax.dynamic_slice_in_dim(vw, s0, NSA_WINDOW + Q_BLOCK, axis=1)
        wpos = s0 - NSA_WINDOW + jnp.arange(NSA_WINDOW + Q_BLOCK)
        wmask = (wpos[None, :] <= tq[:, None]) & (wpos[None, :] > tq[:, None] - NSA_WINDOW) & (wpos[None, :] >= 0)
        p = masked_softmax(jnp.einsum('bqhd,bkd->bhqk', qb, kwb) * scale, wmask)
        o_win = jnp.einsum('bhqk,bkd->bqhd', p.astype(vwb.dtype), vwb)
        return jnp.stack([o_slc, o_win], axis=2)

    o_sw = sweep_query_blocks(block, seq)
    g = jax.nn.sigmoid(gate_pre.astype(jnp.float32)).reshape(bsz, seq, 3, N_HEADS, 1).astype(q.dtype)
    o = g[:, :, 0] * o_cmp + g[:, :, 1] * o_sw[:, :, 0] + g[:, :, 2] * o_sw[:, :, 1]
    return o.reshape(bsz, seq, GROUP_WIDTH)


def diff_mixer(q, k, v, lam_vecs, sub_g, lam_init, cos, sin):
    bsz, seq = q.shape[0], q.shape[1]
    q = apply_rope(q, cos, sin)
    k = apply_rope(k, cos, sin)
    lv = lam_vecs.astype(jnp.float32)
    lam = jnp.exp(jnp.sum(lv[0] * lv[1])) - jnp.exp(jnp.sum(lv[2] * lv[3])) + lam_init
    scale = DIFF_HALF ** -0.5
    kpos = jnp.arange(seq)

    def block(s0):
        qb = lax.dynamic_slice_in_dim(q, s0, Q_BLOCK, axis=1)
        tq = s0 + jnp.arange(Q_BLOCK)
        s = jnp.einsum('bqhcd,bkhcd->bhcqk', qb, k) * scale
        p = masked_softmax(s, kpos[None, :] <= tq[:, None])
        a = p[:, :, 0] - lam * p[:, :, 1]
        return jnp.einsum('bhqk,bkhd->bqhd', a.astype(v.dtype), v)

    o = sweep_query_blocks(block, seq)
    o = rmsnorm(o, sub_g) * (1.0 - lam_init)
    return o.reshape(bsz, seq, GROUP_WIDTH)


def mlstm_mixer(u, v, i_pre, f_pre, o_pre, conv_w, conv_b, wq, wk, gate_b, head_g):
    bsz, seq = u.shape[0], u.shape[1]
    f32 = jnp.float32
    uc = lax.conv_general_dilated(u, conv_w[:, None, :], window_strides=(1,),
                                  padding=[(MLSTM_CONV - 1, 0)],
                                  dimension_numbers=('NWC', 'WIO', 'NWC'),
                                  feature_group_count=GROUP_WIDTH) + conv_b
    uc = jax.nn.silu(uc).reshape(bsz, seq, N_HEADS, HEAD_DIM)
    q = jnp.einsum('bshd,hde->bhse', uc, wq).astype(f32)
    k = jnp.einsum('bshd,hde->bhse', uc, wk).astype(f32) * (HEAD_DIM ** -0.5)
    vv = v.reshape(bsz, seq, N_HEADS, HEAD_DIM).transpose(0, 2, 1, 3).astype(f32)
    gb = gate_b.astype(f32)
    ig = (i_pre.astype(f32) + gb[0]).transpose(0, 2, 1)
    lf = jax.nn.log_sigmoid(f_pre.astype(f32) + gb[1]).transpose(0, 2, 1)

    nc, L = seq // MLSTM_CHUNK, MLSTM_CHUNK
    q = q.reshape(bsz, N_HEADS, nc, L, HEAD_DIM)
    k = k.reshape(bsz, N_HEADS, nc, L, HEAD_DIM)
    vv = vv.reshape(bsz, N_HEADS, nc, L, HEAD_DIM)
    ig = ig.reshape(bsz, N_HEADS, nc, L)
    b = jnp.cumsum(lf.reshape(bsz, N_HEADS, nc, L), axis=-1)
    causal = jnp.tril(jnp.ones((L, L), dtype=bool))
    dmat = jnp.where(causal, b[..., :, None] - b[..., None, :] + ig[..., None, :], NEG_INF)

    a = b[..., -1]
    g_end = a[..., None] - b + ig
    m_loc = jnp.max(g_end, axis=-1)
    w_end = jnp.exp(g_end - m_loc[..., None])
    c_loc = jnp.einsum('bhcl,bhclv,bhclk->bhcvk', w_end, vv, k)
    n_loc = jnp.einsum('bhcl,bhclk->bhck', w_end, k)

    def step(carry, xs):
        c_st, n_st, m_st = carry
        a_c, m_l, c_l, n_l = xs
        m_new = jnp.maximum(a_c + m_st, m_l)
        decay = jnp.exp(a_c + m_st - m_new)
        fresh = jnp.exp(m_l - m_new)
        c_new = decay[..., None, None] * c_st + fresh[..., None, None] * c_l
        n_new = decay[..., None] * n_st + fresh[..., None] * n_l
        return (c_new, n_new, m_new), (c_st, n_st, m_st)

    init = (jnp.zeros((bsz, N_HEADS, HEAD_DIM, HEAD_DIM), f32),
            jnp.zeros((bsz, N_HEADS, HEAD_DIM), f32),
            jnp.zeros((bsz, N_HEADS), f32))
    xs = (jnp.moveaxis(a, 2, 0), jnp.moveaxis(m_loc, 2, 0),
          jnp.moveaxis(c_loc, 2, 0), jnp.moveaxis(n_loc, 2, 0))
    _, (c_in, n_in, m_in) = lax.scan(step, init, xs)
    c_in = jnp.moveaxis(c_in, 0, 2)
    n_in = jnp.moveaxis(n_in, 0, 2)
    m_in = jnp.moveaxis(m_in, 0, 2)

    inter = b + m_in[..., None]
    m_t = jnp.maximum(inter, jnp.max(dmat, axis=-1))
    e_inter = jnp.exp(inter - m_t)
    s_qk = jnp.einsum('bhctd,bhcsd->bhcts', q, k) * jnp.exp(dmat - m_t[..., None])
    num = (e_inter[..., None] * jnp.einsum('bhcvk,bhctk->bhctv', c_in, q)
           + jnp.einsum('bhcts,bhcsv->bhctv', s_qk, vv))
    den = e_inter * jnp.einsum('bhck,bhctk->bhct', n_in, q) + jnp.sum(s_qk, axis=-1)
    h = num / jnp.maximum(jnp.abs(den), jnp.exp(-m_t))[..., None]
    h = h.reshape(bsz, N_HEADS, seq, HEAD_DIM).transpose(0, 2, 1, 3)
    h = rmsnorm(h, head_g.reshape(N_HEADS, HEAD_DIM))
    h = h * jax.nn.sigmoid(o_pre.astype(f32)).reshape(bsz, seq, N_HEADS, HEAD_DIM)
    return h.reshape(bsz, seq, GROUP_WIDTH).astype(u.dtype)


def dilated_mixer(q, k, v, cos, sin):
    bsz, seq = q.shape[0], q.shape[1]
    q = apply_rope(q, cos, sin)
    k = apply_rope(k, cos, sin)
    pad = ((0, 0), (DILATED_PAD, 0), (0, 0), (0, 0))
    kp, vp = jnp.pad(k, pad), jnp.pad(v, pad)
    scale = HEAD_DIM ** -0.5

    def block(s0):
        qb = lax.dynamic_slice_in_dim(q, s0, Q_BLOCK, axis=1)
        tq = s0 + jnp.arange(Q_BLOCK)
        kseg = lax.dynamic_slice_in_dim(kp, s0, DILATED_PAD + Q_BLOCK, axis=1)
        vseg = lax.dynamic_slice_in_dim(vp, s0, DILATED_PAD + Q_BLOCK, axis=1)
        outs, lses = [], []
        for window, dil in DILATED_PATTERNS:
            n_keys = window // dil + 1
            rel = np.arange(Q_BLOCK)[:, None] + DILATED_PAD - np.arange(n_keys)[None, :] * dil
            kg, vg = kseg[:, rel], vseg[:, rel]
            kpos = tq[:, None] - jnp.arange(n_keys)[None, :] * dil
            s = jnp.einsum('bqhd,bqjhd->bhqj', qb, kg) * scale
            p, lse = masked_softmax(s, kpos >= 0, return_lse=True)
            outs.append(jnp.einsum('bhqj,bqjhd->bqhd', p.astype(vg.dtype), vg))
            lses.append(jnp.transpose(lse, (0, 2, 1)))
        alpha = jax.nn.softmax(jnp.stack(lses, axis=0), axis=0)
        return jnp.einsum('gbqh,gbqhd->bqhd', alpha.astype(q.dtype), jnp.stack(outs, axis=0))

    o = sweep_query_blocks(block, seq)
    return o.reshape(bsz, seq, GROUP_WIDTH)


def setup_inputs(seed: int = 0) -> dict:
    key = jax.random.key(seed)
    k = jax.random.split(key, 20)
    f32 = jnp.float32

    def normal(kk, shape, scale):
        return jax.random.normal(kk, shape, f32) * scale

    def gain(kk, shape):
        return 1.0 + 0.02 * jax.random.normal(kk, shape, f32)

    forget_bias = jnp.linspace(3.0, 6.0, N_HEADS, dtype=f32)
    mlstm_gate_b = jnp.stack([normal(k[10], (DEPTH, N_HEADS), 0.1),
                              forget_bias + normal(k[11], (DEPTH, N_HEADS), 0.1)], axis=1)
    return {
        'x': normal(k[0], (BATCH, SEQ, D_MODEL), 1.0),
        'norm_mix': gain(k[1], (DEPTH, D_MODEL)),
        'w_in': normal(k[2], (DEPTH, D_MODEL, D_IN), D_MODEL ** -0.5),
        'nsa_cmp_pos': normal(k[3], (DEPTH, 2, NSA_CMP_LEN, HEAD_DIM), 0.1),
        'nsa_cmp_w': normal(k[4], (DEPTH, 2, NSA_CMP_LEN * HEAD_DIM, HEAD_DIM), (NSA_CMP_LEN * HEAD_DIM) ** -0.5),
        'diff_lambda': normal(k[5], (DEPTH, 4, DIFF_HALF), 0.1),
        'diff_norm': gain(k[6], (DEPTH, HEAD_DIM)),
        'mlstm_conv_w': normal(k[7], (DEPTH, MLSTM_CONV, GROUP_WIDTH), MLSTM_CONV ** -0.5),
        'mlstm_conv_b': normal(k[8], (DEPTH, GROUP_WIDTH), 0.02),
        'mlstm_wq': normal(k[9], (DEPTH, N_HEADS, HEAD_DIM, HEAD_DIM), HEAD_DIM ** -0.5),
        'mlstm_wk': normal(k[12], (DEPTH, N_HEADS, HEAD_DIM, HEAD_DIM), HEAD_DIM ** -0.5),
        'mlstm_gate_b': mlstm_gate_b,
        'mlstm_norm': gain(k[13], (DEPTH, GROUP_WIDTH)),
        'w_out': normal(k[14], (DEPTH, D_MODEL, D_MODEL), D_MODEL ** -0.5),
        'norm_ffn': gain(k[15], (DEPTH, D_MODEL)),
        'w_gate': normal(k[16], (DEPTH, D_MODEL, D_FF), D_MODEL ** -0.5),
        'w_up': normal(k[17], (DEPTH, D_MODEL, D_FF), D_MODEL ** -0.5),
        'w_down': normal(k[18], (DEPTH, D_FF, D_MODEL), D_FF ** -0.5),
        'norm_final': gain(k[19], (D_MODEL,)),
    }


def reference(x, norm_mix, w_in, nsa_cmp_pos, nsa_cmp_w, diff_lambda, diff_norm,
              mlstm_conv_w, mlstm_conv_b, mlstm_wq, mlstm_wk, mlstm_gate_b, mlstm_norm,
              w_out, norm_ffn, w_gate, w_up, w_down, norm_final):
    bsz, seq, _ = x.shape
    cos64, sin64 = rope_tables(seq, HEAD_DIM)
    cos32, sin32 = rope_tables(seq, DIFF_HALF)
    offsets = [int(o) for o in np.cumsum(IN_SPLITS)[:-1]]

    def heads(t):
        return t.reshape(bsz, seq, N_HEADS, HEAD_DIM)

    for layer in range(DEPTH):
        h = rmsnorm(x, norm_mix[layer])
        z = h @ w_in[layer]
        (a_q, a_kc, a_vc, a_ks, a_vs, a_kw, a_vw, a_g,
         b_q, b_k, b_v, c_u, c_v, c_i, c_f, c_o, d_q, d_k, d_v) = jnp.split(z, offsets, axis=-1)
        o_a = nsa_mixer(heads(a_q), a_kc, a_vc, a_ks, a_vs, a_kw, a_vw, a_g,
                        nsa_cmp_pos[layer], nsa_cmp_w[layer], cos64, sin64)
        lam_init = 0.8 - 0.6 * math.exp(-0.3 * layer)
        o_b = diff_mixer(b_q.reshape(bsz, seq, N_HEADS, 2, DIFF_HALF),
                         b_k.reshape(bsz, seq, N_HEADS, 2, DIFF_HALF), heads(b_v),
                         diff_lambda[layer], diff_norm[layer], lam_init, cos32, sin32)
        o_c = mlstm_mixer(c_u, c_v, c_i, c_f, c_o, mlstm_conv_w[layer], mlstm_conv_b[layer],
                          mlstm_wq[layer], mlstm_wk[layer], mlstm_gate_b[layer], mlstm_norm[layer])
        o_d = dilated_mixer(heads(d_q), heads(d_k), heads(d_v), cos64, sin64)
        x = x + jnp.concatenate([o_a, o_b, o_c, o_d], axis=-1) @ w_out[layer]
        h = rmsnorm(x, norm_ffn[layer])
        x = x + (jax.nn.silu(h @ w_gate[layer]) * (h @ w_up[layer])) @ w_down[layer]
    return rmsnorm(x, norm_final)
```

```python
import numpy as np
import concourse.bass as bass
import concourse.mybir as mybir
from contextlib import ExitStack
import ml_dtypes
from concourse.bass_utils import run_bass_kernel_spmd

F32 = mybir.dt.float32
BF16 = mybir.dt.bfloat16
AF = mybir.ActivationFunctionType
ALU = mybir.AluOpType
AX = mybir.AxisListType

N_DMA_SEMS = 6


class Buf:
    __slots__ = ("t", "w", "r", "name", "excl")

    def __init__(self, t=None, name="", excl=False):
        self.t = t
        self.excl = excl
        self.w = None
        self.r = {}
        self.name = name

    def __getitem__(self, idx):
        return self.t[idx]


class Prog:
    ENG = ("pe", "dve", "act", "pool", "sp")

    def __init__(self, name="k"):
        self.nc = bass.Bass("TRN2", target_bir_lowering=False)
        self.es = ExitStack()
        self.ops = {e: [] for e in self.ENG}
        self.cnt = {e: 0 for e in self.ENG}
        self.waited = {e: {} for e in self.ENG}
        self.sems = {}
        self.dma_k = {}
        self.nbuf = 0
        for e in ("pe", "dve", "act", "pool"):
            self.sems[e] = self.es.enter_context(self.nc.semaphore("s_" + e))
        for q in ("sp", "act", "pool"):
            self.dma_k[q] = 0
            for i in range(N_DMA_SEMS):
                self.sems[("dma", q, i)] = self.es.enter_context(self.nc.semaphore(f"d_{q}_{i}"))

    def sb(self, shape, dt, name=None):
        self.nbuf += 1
        name = name or f"sb{self.nbuf}"
        t = self.es.enter_context(self.nc.sbuf_tensor(name, list(shape), dt))
        return Buf(t, name)

    def ps(self, shape, dt=F32, name=None):
        self.nbuf += 1
        name = name or f"ps{self.nbuf}"
        t = self.es.enter_context(self.nc.psum_tensor(name, list(shape), dt))
        return Buf(t, name, excl=True)

    def dram(self, name, shape, dt, kind="Internal"):
        t = self.nc.dram_tensor(name, list(shape), dt, kind=kind)
        return Buf(t, name)

    def _deps(self, eng, reads, writes):
        need = {}

        def add(tok):
            if tok is None:
                return
            k, v = tok
            if need.get(k, 0) < v:
                need[k] = v

        for b in reads:
            add(b.w)
            if b.excl:
                for k, v in b.r.items():
                    if k != eng:
                        add((k, v))
        for b in writes:
            add(b.w)
            for k, v in b.r.items():
                add((k, v))
        waits = []
        wd = self.waited[eng]
        for k, v in need.items():
            if eng == "pe" and k == "pe":
                continue
            if wd.get(k, 0) >= v:
                continue
            wd[k] = v
            waits.append((k, v))
        return waits

    def _mark(self, tok, reads, writes):
        k, v = tok
        for b in reads:
            if b.r.get(k, 0) < v:
                b.r[k] = v
        for b in writes:
            b.w = tok
            b.r = {}

    def op(self, eng, fn, reads=(), writes=()):
        waits = self._deps(eng, reads, writes)
        self.cnt[eng] += 1
        tok = (eng, self.cnt[eng])
        self._mark(tok, reads, writes)
        self.ops[eng].append((waits, fn, eng))

    def dma(self, q, out, in_, reads=(), writes=(), **kw):
        k = self.dma_k[q]
        self.dma_k[q] = k + 1
        slot = k % N_DMA_SEMS
        key = ("dma", q, slot)
        val = 16 * (k // N_DMA_SEMS + 1)
        waits = self._deps(q, reads, writes)
        prev = val - 16
        if prev > 0 and self.waited[q].get(key, 0) < prev:
            self.waited[q][key] = prev
            waits.append((key, prev))
        self._mark((key, val), reads, writes)
        self.ops[q].append((waits, lambda e: e.dma_start(out=out, in_=in_, **kw), key))

    def barrier(self):
        toks = [(e, self.cnt[e]) for e in ("pe", "dve", "act", "pool") if self.cnt[e] > 0]
        for q in ("sp", "act", "pool"):
            k = self.dma_k[q]
            for slot in range(N_DMA_SEMS):
                n = (k - slot + N_DMA_SEMS - 1) // N_DMA_SEMS
                if n > 0:
                    toks.append((("dma", q, slot), 16 * n))
        for eng in self.ENG:
            waits = []
            for k, v in toks:
                if self.waited[eng].get(k, 0) < v:
                    self.waited[eng][k] = v
                    waits.append((k, v))
            if waits:
                self.ops[eng].append((waits, None, None))

    def simulate(self):
        sem = {}
        ptr = {e: 0 for e in self.ENG}
        while True:
            prog = False
            done = True
            for e in self.ENG:
                lst = self.ops[e]
                while ptr[e] < len(lst):
                    done = False
                    waits, fn, inc = lst[ptr[e]]
                    if any(sem.get(k, 0) < v for k, v in waits):
                        break
                    if fn is not None and inc is not None:
                        sem[inc] = sem.get(inc, 0) + (16 if isinstance(inc, tuple) else 1)
                    ptr[e] += 1
                    prog = True
            if all(ptr[e] == len(self.ops[e]) for e in self.ENG):
                return True
            if not prog:
                for e in self.ENG:
                    if ptr[e] < len(self.ops[e]):
                        waits = self.ops[e][ptr[e]][0]
                        print("DEADLOCK", e, ptr[e], len(self.ops[e]), [(k, v, sem.get(k, 0)) for k, v in waits if sem.get(k, 0) < v])
                return False

    def finish(self):
        self.barrier()
        if not self.simulate():
            raise RuntimeError("semaphore protocol deadlock")
        nc = self.nc
        sems = self.sems
        ops = self.ops

        def run(e, lst):
            for waits, fn, inc in lst:
                for k, v in waits:
                    e.wait_ge(sems[k], v)
                if fn is None:
                    continue
                ins = fn(e)
                if inc is None:
                    continue
                if isinstance(inc, tuple):
                    ins.then_inc(sems[inc], 16)
                else:
                    ins.then_inc(sems[inc], 1)

        with nc.Block() as block:
            @block.tensor
            def _(e):
                run(e, ops["pe"])

            @block.vector
            def _(e):
                run(e, ops["dve"])

            @block.scalar
            def _(e):
                run(e, ops["act"])

            @block.gpsimd
            def _(e):
                run(e, ops["pool"])

            @block.sync
            def _(e):
                run(e, ops["sp"])
        self.es.close()
        return nc


D = 1024
DFF = 2816
NF = DFF // 128
EPS = 1e-6


def make_ident(P, dt=BF16):
    ident = P.sb([128, 128], dt, "ident")
    P.op("pool", lambda e: e.memset(ident[:], 0.0), writes=[ident])
    P.op("pool", lambda e: e.affine_select(out=ident[:], in_=ident[:], pattern=[[-1, 128]],
                                           compare_op=ALU.not_equal, fill=1.0, base=0, channel_multiplier=1),
         reads=[ident], writes=[ident])
    return ident


def rms_rstd(P, xt, xb, ss, ssb, junk, junkb, width=D):
    P.op("act", lambda e: e.activation(out=junk[:, 0:width], in_=xt, func=AF.Square, accum_out=ss[:, 0:1]),
         reads=[xb], writes=[junkb, ssb])
    P.op("dve", lambda e: e.tensor_scalar(out=ss[:, 0:1], in0=ss[:, 0:1], scalar1=1.0 / width, scalar2=EPS,
                                          op0=ALU.mult, op1=ALU.add), reads=[ssb], writes=[ssb])
    P.op("act", lambda e: e.activation(out=ss[:, 0:1], in_=ss[:, 0:1], func=AF.Sqrt), reads=[ssb], writes=[ssb])
    P.op("dve", lambda e: e.reciprocal(out=ss[:, 0:1], in_=ss[:, 0:1]), reads=[ssb], writes=[ssb])


def build_ffn(last, ntok=2048):
    P = Prog()
    NT = ntok // 128
    xin = P.dram("xin", [ntok, D], F32, "ExternalInput")
    oT = P.dram("oT", [D, ntok], BF16, "ExternalInput")
    w_out = P.dram("w_out", [D, D], F32, "ExternalInput")
    w_gate = P.dram("w_gate", [D, DFF], F32, "ExternalInput")
    w_up = P.dram("w_up", [D, DFF], F32, "ExternalInput")
    w_down = P.dram("w_down", [DFF, D], F32, "ExternalInput")
    gf = P.dram("gf", [1, D], F32, "ExternalInput")
    gl = P.dram("gl", [1, D], F32, "ExternalInput")
    xout = P.dram("xout", [ntok, D], F32, "ExternalOutput")

    wo = P.sb([128, 8, D], BF16, "wo")
    wg = P.sb([128, 8, DFF], BF16, "wg")
    wu = P.sb([128, 8, DFF], BF16, "wu")
    wd = P.sb([128, NF, D], BF16, "wd")
    gfb = P.sb([128, D], F32, "gfb")
    glb = P.sb([128, D], F32, "glb")
    ident = make_ident(P)
    wo_c = [Buf() for _ in range(8)]
    wg_c = [Buf() for _ in range(8)]
    wu_c = [Buf() for _ in range(8)]
    wd_c = [Buf() for _ in range(NF)]
    P.dma("sp", gfb[:], gf[0:1, :].partition_broadcast(128), writes=[gfb])
    P.dma("sp", glb[:], gl[0:1, :].partition_broadcast(128), writes=[glb])
    for c in range(8):
        P.dma("pool", wo[:, c, :], w_out[c * 128:(c + 1) * 128, :], writes=[wo_c[c]])
    for c in range(8):
        P.dma("pool", wg[:, c, :], w_gate[c * 128:(c + 1) * 128, :], writes=[wg_c[c]], max_dma_last_dim=4096)
        P.dma("pool", wu[:, c, :], w_up[c * 128:(c + 1) * 128, :], writes=[wu_c[c]], max_dma_last_dim=4096)
    for f in range(NF):
        P.dma("pool", wd[:, f, :], w_down[f * 128:(f + 1) * 128, :], writes=[wd_c[f]])

    oTs = P.sb([128, 8, 256], BF16, "oTs")
    xt = [P.sb([128, D], F32, f"xt{i}") for i in range(2)]
    h = P.sb([128, D], BF16, "h")
    hT = P.sb([128, 8, 256], BF16, "hT")
    aT = P.sb([128, NF, 256], BF16, "aT")
    sg = [P.sb([128, 256], F32, f"sg{i}") for i in range(2)]
    xo = [P.sb([128, D], F32, f"xo{i}") for i in range(2)]
    junk = P.sb([128, D], F32, "junk")
    ss = P.sb([128, 4], F32, "ss")
    py = [P.ps([128, 512], F32, f"py{i}") for i in range(2)]
    pT = P.ps([128, 1024], BF16, "pT")
    pg = [P.ps([128, 512], F32, f"pg{i}") for i in range(2)]
    pu = [P.ps([128, 512], F32, f"pu{i}") for i in range(2)]
    oT_v = oT.t.ap().rearrange("(c p) t -> p c t", p=128)

    for s in range(NT // 2):
        P.dma("sp", oTs[:], oT_v[:, :, s * 256:(s + 1) * 256], writes=[oTs])
        for jj in range(2):
            j = 2 * s + jj
            x_ = xt[jj]
            P.dma("sp", x_[:], xin[j * 128:(j + 1) * 128, :], writes=[x_])
            for hf in range(2):
                for c in range(8):
                    P.op("pe", lambda e, hf=hf, c=c, jj=jj: e.matmul(
                        py[hf][:], lhsT=oTs[:, c, jj * 128:(jj + 1) * 128], rhs=wo[:, c, hf * 512:(hf + 1) * 512],
                        start=(c == 0), stop=(c == 7)), reads=[oTs, wo_c[c]], writes=[py[hf]])
                P.op("dve", lambda e, hf=hf, x_=x_: e.tensor_tensor(
                    out=x_[:, hf * 512:(hf + 1) * 512], in0=py[hf][:], in1=x_[:, hf * 512:(hf + 1) * 512], op=ALU.add),
                    reads=[py[hf], x_], writes=[x_])
            rms_rstd(P, x_[:], x_, ss, ss, junk, junk)
            P.op("dve", lambda e, x_=x_: e.scalar_tensor_tensor(
                out=h[:], in0=x_[:], scalar=ss[:, 0:1], in1=gfb[:], op0=ALU.mult, op1=ALU.mult),
                reads=[x_, ss, gfb], writes=[h])
            for c in range(8):
                P.op("pe", lambda e, c=c: e.transpose(out=pT[:, c * 128:(c + 1) * 128], in_=h[:, c * 128:(c + 1) * 128],
                                                     identity=ident[:]), reads=[h, ident], writes=[pT])
            P.op("act", lambda e, jj=jj: e.copy(out=hT[:, :, jj * 128:(jj + 1) * 128],
                                               in_=pT[:].rearrange("p (c t) -> p c t", c=8)),
                 reads=[pT], writes=[hT])
        for f in range(NF):
            g_, u_, s_ = pg[f % 2], pu[f % 2], sg[f % 2]
            for c in range(8):
                P.op("pe", lambda e, f=f, c=c, g_=g_: e.matmul(
                    g_[:, 0:256], lhsT=wg[:, c, f * 128:(f + 1) * 128], rhs=hT[:, c, :], start=(c == 0), stop=(c == 7)),
                    reads=[wg_c[c], hT], writes=[g_])
            for c in range(8):
                P.op("pe", lambda e, f=f, c=c, u_=u_: e.matmul(
                    u_[:, 0:256], lhsT=wu[:, c, f * 128:(f + 1) * 128], rhs=hT[:, c, :], start=(c == 0), stop=(c == 7)),
                    reads=[wu_c[c], hT], writes=[u_])
            P.op("act", lambda e, g_=g_, s_=s_: e.activation(out=s_[:], in_=g_[:, 0:256], func=AF.Silu),
                 reads=[g_], writes=[s_])
            P.op("dve", lambda e, f=f, u_=u_, s_=s_: e.tensor_tensor(out=aT[:, f, :], in0=s_[:], in1=u_[:, 0:256], op=ALU.mult),
                 reads=[s_, u_], writes=[aT])
        for jj in range(2):
            j = 2 * s + jj
            x_ = xt[jj]
            o_ = xo[jj]
            for hf in range(2):
                for f in range(NF):
                    P.op("pe", lambda e, hf=hf, f=f, jj=jj: e.matmul(
                        py[hf][:], lhsT=aT[:, f, jj * 128:(jj + 1) * 128], rhs=wd[:, f, hf * 512:(hf + 1) * 512],
                        start=(f == 0), stop=(f == NF - 1)), reads=[aT, wd_c[f]], writes=[py[hf]])
                P.op("dve", lambda e, hf=hf, x_=x_, o_=o_: e.tensor_tensor(
                    out=o_[:, hf * 512:(hf + 1) * 512], in0=py[hf][:], in1=x_[:, hf * 512:(hf + 1) * 512], op=ALU.add),
                    reads=[py[hf], x_], writes=[o_])
            if last:
                rms_rstd(P, o_[:], o_, ss, ss, junk, junk)
                P.op("dve", lambda e, o_=o_: e.scalar_tensor_tensor(
                    out=o_[:], in0=o_[:], scalar=ss[:, 0:1], in1=glb[:], op0=ALU.mult, op1=ALU.mult),
                    reads=[o_, ss, glb], writes=[o_])
            P.dma("sp", xout[j * 128:(j + 1) * 128, :], o_[:], reads=[o_], writes=[xout])
    return P.finish()


S = 4096
NTT = S // 128
LAM_INIT = [0.8 - 0.6 * float(np.exp(-0.3 * l)) for l in range(2)]


import os
STOP = int(os.environ.get("KSTOP", "0"))


class Arena:
    def __init__(self, P, words):
        self.P = P
        self.t = P.sb([128, words], F32, "arena")
        self.words = words
        self.off = 0

    def alloc(self, free_shape, dt, name=""):
        n = int(np.prod(free_shape))
        w = (n * (2 if dt == BF16 else 4) + 3) // 4
        w = (w + 7) // 8 * 8
        assert self.off + w <= self.words, (name, self.off, w, self.words)
        v = self.t[:, self.off:self.off + w]
        self.off += w
        if dt == BF16:
            v = v.bitcast(BF16)
        v = v[:, 0:n]
        if len(free_shape) == 2:
            v = v.rearrange("p (a b) -> p a b", a=free_shape[0])
        elif len(free_shape) == 3:
            v = v.rearrange("p (a b c) -> p a b c", a=free_shape[0], b=free_shape[1])
        return Buf(v, name)

    def mark(self):
        return self.off

    def reset(self, m):
        self.P.barrier()
        self.off = m


class Cx:
    pass


def mm(P, out, lhsT, rhs, start, stop, R, W, skip=False):
    if skip:
        P.op("pe", lambda e: e.matmul(out, lhsT=lhsT, rhs=rhs, start=start, stop=stop, skip_group_check=True), reads=R, writes=W)
    else:
        P.op("pe", lambda e: e.matmul(out, lhsT=lhsT, rhs=rhs, start=start, stop=stop), reads=R, writes=W)


def tt(P, eng, out, in0, in1, op, R, W):
    P.op(eng, lambda e: e.tensor_tensor(out=out, in0=in0, in1=in1, op=op), reads=R, writes=W)


def ts(P, eng, out, in0, s1, s2, op0, op1, R, W):
    if s2 is None:
        P.op(eng, lambda e: e.tensor_scalar(out=out, in0=in0, scalar1=s1, scalar2=None, op0=op0), reads=R, writes=W)
    else:
        P.op(eng, lambda e: e.tensor_scalar(out=out, in0=in0, scalar1=s1, scalar2=s2, op0=op0, op1=op1), reads=R, writes=W)


def stt(P, out, in0, scalar, in1, op0, op1, R, W):
    P.op("dve", lambda e: e.scalar_tensor_tensor(out=out, in0=in0, scalar=scalar, in1=in1, op0=op0, op1=op1), reads=R, writes=W)


def actf(P, out, in_, func, R, W, scale=1.0, accum=None):
    if accum is None:
        P.op("act", lambda e: e.activation(out=out, in_=in_, func=func, scale=scale), reads=R, writes=W)
    else:
        P.op("act", lambda e: e.activation(out=out, in_=in_, func=func, scale=scale, accum_out=accum), reads=R, writes=W)


def load_w(P, cx, wdram, ncols, name):
    W = cx.ar.alloc((8, ncols), BF16, name)
    for c in range(8):
        P.dma("pool", W[:, c, :], wdram[c * 128:(c + 1) * 128, :], writes=[W])
    return W


class Rot:
    def __init__(self, bufs):
        self.b = bufs
        self.i = 0

    def get(self):
        b = self.b[self.i % len(self.b)]
        self.i += 1
        return b


def proj_fm(P, cx, W, col0, nrows, evac):
    for tb in range(8):
        ps = cx.pp.get()
        for c in range(8):
            mm(P, ps[0:nrows, :], W[:, c, col0:col0 + nrows], cx.hT[:, c, tb * 512:(tb + 1) * 512], c == 0, c == 7, [W, cx.hT], [ps])
        evac(ps, tb)


def proj_tm(P, cx, W, col0, ncols, j, ps, tok_ap=None):
    for c in range(8):
        lhsT = cx.hT[:, c, j * 128:(j + 1) * 128] if tok_ap is None else tok_ap(c)
        mm(P, ps[:, 0:ncols], lhsT, W[:, c, col0:col0 + ncols], c == 0, c == 7, [W, cx.hT], [ps])


def rope_evac(P, cx, ps, tb, nrows, dst, pswapT, CC, SS, dst_b=None, raw_dst=None):
    if STOP == 121:
        return
    sl = slice(tb * 512, (tb + 1) * 512)
    if raw_dst is None:
        qs = cx.ropeq.get()
    else:
        qs = Buf(raw_dst[:, sl], "rawv")
        qs.w, qs.r = raw_dst.w, raw_dst.r
    t1 = cx.ropet.get()
    P.op("act", lambda e: e.copy(out=qs[0:nrows, :], in_=ps[0:nrows, :]), reads=[ps], writes=[qs] if raw_dst is None else [raw_dst])
    if STOP == 122:
        return
    ps2 = cx.pp.get()
    mm(P, ps2[0:nrows, :], pswapT[0:nrows, 0:nrows], qs[0:nrows, :], True, True, [pswapT, qs if raw_dst is None else raw_dst], [ps2])
    if STOP == 123:
        return
    if STOP == 1241:
        tt(P, "dve", t1[0:nrows, :], ps[0:nrows, :], qs[0:nrows, :], ALU.mult, [ps, qs], [t1])
        return
    if STOP == 1242:
        tt(P, "dve", t1[0:nrows, :], CC[0:nrows, sl], CC[0:nrows, sl], ALU.mult, [CC], [t1])
        return
    tt(P, "dve", t1[0:nrows, :], ps[0:nrows, :], CC[0:nrows, sl], ALU.mult, [ps, CC], [t1])
    if STOP == 1243:
        return
    if STOP == 1244:
        tt(P, "dve", t1[0:nrows, :], CC[0:nrows, sl], ps[0:nrows, :], ALU.mult, [ps, CC], [t1])
        return
    t2 = cx.ropet.get()
    tt(P, "dve", t2[0:nrows, :], ps2[0:nrows, :], SS[0:nrows, sl], ALU.mult, [ps2, SS], [t2])
    if STOP == 124:
        return
    tt(P, "pool", dst[0:nrows, sl], t1[0:nrows, :], t2[0:nrows, :], ALU.add, [t1, t2], [dst if dst_b is None else dst_b])


def build_consts(P, cx):
    ar = cx.ar
    cx.ident = ar.alloc((128,), BF16, "ident")
    idn = cx.ident
    P.op("pool", lambda e: e.memset(idn[:], 0.0), writes=[idn])
    P.op("pool", lambda e: e.affine_select(out=idn[:], in_=idn[:], pattern=[[-1, 128]], compare_op=ALU.not_equal,
                                           fill=1.0, base=0, channel_multiplier=1), reads=[idn], writes=[idn])

    def tri(name, pat, cm, cmp):
        m = ar.alloc((128,), BF16, name)
        P.op("pool", lambda e: e.memset(m[:], 1.0), writes=[m])
        P.op("pool", lambda e: e.affine_select(out=m[:], in_=m[:], pattern=[[pat, 128]], compare_op=cmp, fill=0.0,
                                               base=0, channel_multiplier=cm), reads=[m], writes=[m])
        return m
    cx.TRI = tri("TRI", 1, -1, ALU.is_ge)
    cx.ATRI_S = tri("ATRIS", -1, 1, ALU.is_gt)
    cx.ATRI_GE = tri("ATRIGE", -1, 1, ALU.is_ge)


def stage0(P, cx, x, gm):
    ar = cx.ar
    m = ar.mark()
    gb = ar.alloc((D,), F32, "gmb")
    P.dma("sp", gb[:], gm[0:1, :].partition_broadcast(128), writes=[gb])
    xt = [ar.alloc((D,), F32, f"s0x{i}") for i in range(2)]
    hb = [ar.alloc((D,), BF16, f"s0h{i}") for i in range(2)]
    junk = ar.alloc((D,), F32, "s0junk")
    ssr = [ar.alloc((4,), F32, f"s0ss{i}") for i in range(2)]
    for j in range(NTT):
        x_, h_, ss = xt[j % 2], hb[j % 2], ssr[j % 2]
        P.dma("sp", x_[:], x[j * 128:(j + 1) * 128, :], writes=[x_])
        rms_rstd(P, x_[:], x_, ss, ss, junk, junk)
        stt(P, h_[:], x_[:], ss[:, 0:1], gb[:], ALU.mult, ALU.mult, [x_, ss, gb], [h_])
        pt = cx.ptp.get()
        ptv = pt[:].bitcast(BF16)
        for c in range(8):
            P.op("pe", lambda e, c=c, ptv=ptv, h_=h_: e.transpose(out=ptv[:, c * 128:(c + 1) * 128], in_=h_[:, c * 128:(c + 1) * 128],
                                                                 identity=cx.ident[:]), reads=[h_, cx.ident], writes=[pt])
        P.op("act", lambda e, j=j, ptv=ptv: e.copy(out=cx.hT[:, :, j * 128:(j + 1) * 128], in_=ptv.rearrange("p (c t) -> p c t", c=8)),
             reads=[pt], writes=[cx.hT])
    ar.reset(m)


def mixer_B(P, cx, wb, lamd, dnorm, rope32, pswap32, oT, layer):
    ar = cx.ar
    m0 = ar.mark()
    W = load_w(P, cx, wb, 384, "WB")
    CC = ar.alloc((S,), F32, "CC")
    SS = ar.alloc((S,), F32, "SS")
    P.dma("sp", CC[:], rope32[0], writes=[CC])
    P.dma("sp", SS[:], rope32[1], writes=[SS])
    psw = ar.alloc((128,), BF16, "psw")
    P.dma("pool", psw[:], pswap32[:, :], writes=[psw])
    qT = [ar.alloc((S,), BF16, f"qT{i}") for i in range(2)]
    kT = [ar.alloc((S,), BF16, f"kT{i}") for i in range(2)]
    V = ar.alloc((NTT, 2, 65), BF16, "V")
    res = ar.alloc((NTT, 4, 64), F32, "res")
    oTs = ar.alloc((S,), BF16, "oTs")
    cx.ropeq = Rot([ar.alloc((512,), BF16, f"rq{i}") for i in range(2)])
    cx.ropet = Rot([ar.alloc((512,), F32, f"rt{i}") for i in range(3)])
    Es = Rot([ar.alloc((512,), BF16, f"E{i}") for i in range(3)])
    lv = ar.alloc((128,), F32, "lv")
    lsm = ar.alloc((8,), F32, "lsm")
    P.dma("sp", lv[:], lamd[0:1, :].partition_broadcast(128), writes=[lv])
    lp = ar.alloc((64,), F32, "lp")
    tt(P, "dve", lp[:, 0:32], lv[:, 0:32], lv[:, 32:64], ALU.mult, [lv], [lp])
    tt(P, "dve", lp[:, 32:64], lv[:, 64:96], lv[:, 96:128], ALU.mult, [lv], [lp])
    P.op("dve", lambda e: e.tensor_reduce(out=lsm[:, 0:2], in_=lp[:].rearrange("p (a b) -> p a b", a=2), axis=AX.X, op=ALU.add),
         reads=[lp], writes=[lsm])
    actf(P, lsm[:, 2:4], lsm[:, 0:2], AF.Exp, [lsm], [lsm])
    tt(P, "dve", lsm[:, 4:5], lsm[:, 3:4], lsm[:, 2:3], ALU.subtract, [lsm], [lsm])
    ts(P, "dve", lsm[:, 5:6], lsm[:, 4:5], -LAM_INIT[layer], None, ALU.add, None, [lsm], [lsm])
    gB = ar.alloc((128,), F32, "gB")
    P.dma("sp", gB[:, 0:64], dnorm[0:1, :].partition_broadcast(128), writes=[gB])
    P.dma("sp", gB[:, 64:128], dnorm[0:1, :].partition_broadcast(128), writes=[gB])
    ts(P, "dve", gB[:], gB[:], 1.0 - LAM_INIT[layer], None, ALU.mult, None, [gB], [gB])
    if STOP == 1:
        return
    if os.environ.get("KBAR"):
        P.barrier()
    for hl in range(2):
        proj_fm(P, cx, W, hl * 64, 64, lambda ps, tb, hl=hl: rope_evac(P, cx, ps, tb, 64, qT[hl], psw, CC, SS))
        proj_fm(P, cx, W, 128 + hl * 64, 64, lambda ps, tb, hl=hl: rope_evac(P, cx, ps, tb, 64, kT[hl], psw, CC, SS))
    if STOP in (12, 121, 122, 123, 124, 1241, 1242, 1243, 1244):
        return
    P.op("pool", lambda e: e.memset(V[:], 1.0), writes=[V])
    if STOP == 13:
        return
    for j in range(NTT):
        ps = cx.pp.get()
        proj_tm(P, cx, W, 256, 128, j, ps)
        P.op("act", lambda e, j=j, ps=ps: e.copy(out=V[:, j, :, 0:64], in_=ps[:, 0:128].rearrange("p (h d) -> p h d", h=2)),
             reads=[ps], writes=[V])
    if STOP == 2:
        return
    sc = 32.0 ** -0.5
    rr = Rot([ar.alloc((4,), F32, f"rr{i}") for i in range(2)])
    for hl in range(2):
        for c in range(2):
            r0 = c * 32
            for QB in range(8):
                acc = cx.pacc.get()
                first = True
                for kt in range(4 * QB + 4):
                    c0 = max(0, kt - 4 * QB) * 128
                    n = 512 - c0
                    Sb = cx.pS.get()
                    mm(P, Sb[:, 0:n], kT[hl][r0:r0 + 32, kt * 128:(kt + 1) * 128], qT[hl][r0:r0 + 32, QB * 512 + c0:(QB + 1) * 512],
                       True, True, [kT[hl], qT[hl]], [Sb])
                    E = Es.get()
                    actf(P, E[:, 0:n], Sb[:, 0:n], AF.Exp, [Sb], [E], scale=sc)
                    if kt >= 4 * QB:
                        tt(P, "pool", E[:, 0:128], E[:, 0:128], cx.TRI[:], ALU.mult, [E, cx.TRI], [E])
                    for qt in range(max(kt, 4 * QB), 4 * QB + 4):
                        ql = qt - 4 * QB
                        ec = ql * 128 - c0
                        mm(P, acc[:, ql * 65:(ql + 1) * 65], E[:, ec:ec + 128], V[:, kt, hl, :], first, kt == qt, [E, V], [acc], skip=True)
                        first = False
                r_ = rr.get()
                accv = acc[:, 0:260].rearrange("p (a b) -> p a b", a=4)
                P.op("dve", lambda e, r_=r_, accv=accv: e.reciprocal(out=r_[:, 0:4], in_=accv[:, :, 64]), reads=[acc], writes=[r_])
                for ql in range(4):
                    ts(P, "dve", res[:, 4 * QB + ql, hl * 2 + c, :], acc[:, ql * 65:ql * 65 + 64], r_[:, ql:ql + 1], None, ALU.mult, None,
                       [acc, r_], [res])
    if STOP == 3:
        return
    a_ = Rot([ar.alloc((128,), F32, f"a{i}") for i in range(2)])
    sq_ = Rot([ar.alloc((128,), F32, f"sq{i}") for i in range(2)])
    s2_ = Rot([ar.alloc((4,), F32, f"s2{i}") for i in range(2)])
    an_ = Rot([ar.alloc((128,), BF16, f"an{i}") for i in range(2)])
    for j in range(NTT):
        a, sq, s2, an = a_.get(), sq_.get(), s2_.get(), an_.get()
        for hl in range(2):
            stt(P, a[:, hl * 64:(hl + 1) * 64], res[:, j, hl * 2 + 1, :], lsm[:, 5:6], res[:, j, hl * 2, :], ALU.mult, ALU.add, [res, lsm], [a])
        tt(P, "dve", sq[:], a[:], a[:], ALU.mult, [a], [sq])
        P.op("dve", lambda e, s2=s2, sq=sq: e.tensor_reduce(out=s2[:, 0:2], in_=sq[:].rearrange("p (a b) -> p a b", a=2), axis=AX.X, op=ALU.add),
             reads=[sq], writes=[s2])
        ts(P, "dve", s2[:, 0:2], s2[:, 0:2], 1.0 / 64, EPS, ALU.mult, ALU.add, [s2], [s2])
        actf(P, s2[:, 0:2], s2[:, 0:2], AF.Sqrt, [s2], [s2])
        P.op("dve", lambda e, s2=s2: e.reciprocal(out=s2[:, 0:2], in_=s2[:, 0:2]), reads=[s2], writes=[s2])
        for hl in range(2):
            stt(P, an[:, hl * 64:(hl + 1) * 64], a[:, hl * 64:(hl + 1) * 64], s2[:, hl:hl + 1], gB[:, hl * 64:(hl + 1) * 64], ALU.mult, ALU.mult,
                [a, s2, gB], [an])
        pt = cx.ptp.get()
        ptv = pt[:].bitcast(BF16)
        P.op("pe", lambda e, ptv=ptv, an=an: e.transpose(out=ptv[:, 0:128], in_=an[:], identity=cx.ident[:]), reads=[an, cx.ident], writes=[pt])
        P.op("act", lambda e, ptv=ptv, j=j: e.copy(out=oTs[:, j * 128:(j + 1) * 128], in_=ptv[:, 0:128]), reads=[pt], writes=[oTs])
    P.dma("sp", oT[128:256, :], oTs[:], reads=[oTs], writes=[oT])
    ar.reset(m0)


def mixer_D(P, cx, wd, rope64, pswap64, oT):
    ar = cx.ar
    m0 = ar.mark()
    W = load_w(P, cx, wd, 384, "WD")
    qT = ar.alloc((S,), BF16, "dqT")
    kT = ar.alloc((S,), BF16, "dkT")
    Vp = [ar.alloc((NTT, 2, 65), BF16, f"dV{p}") for p in range(3)]
    m1 = ar.mark()
    CC = ar.alloc((S,), F32, "CC")
    SS = ar.alloc((S,), F32, "SS")
    P.dma("sp", CC[:], rope64[0], writes=[CC])
    P.dma("sp", SS[:], rope64[1], writes=[SS])
    psw = ar.alloc((128,), BF16, "psw")
    P.dma("pool", psw[:], pswap64[:, :], writes=[psw])
    cx.ropeq = Rot([ar.alloc((512,), BF16, f"rq{i}") for i in range(2)])
    cx.ropet = Rot([ar.alloc((512,), F32, f"rt{i}") for i in range(3)])
    proj_fm(P, cx, W, 0, 128, lambda ps, tb: rope_evac(P, cx, ps, tb, 128, qT, psw, CC, SS))
    proj_fm(P, cx, W, 128, 128, lambda ps, tb: rope_evac(P, cx, ps, tb, 128, kT, psw, CC, SS))
    DIL = (1, 4, 16)
    for p, dd in enumerate(DIL):
        V = Vp[p]
        P.op("pool", lambda e, V=V: e.memset(V[:], 1.0), writes=[V])
        tpc = NTT // dd
        for r in range(dd):
            for a in range(tpc):
                tid = r * tpc + a
                st = dd * 128 * a + r
                ps = cx.pp.get()
                proj_tm(P, cx, W, 256, 128, None, ps, tok_ap=lambda c, st=st, dd=dd: cx.hT[:, c, st:st + dd * 127 + 1:dd])
                P.op("act", lambda e, tid=tid, ps=ps, V=V: e.copy(out=V[:, tid, :, 0:64], in_=ps[:, 0:128].rearrange("p (h d) -> p h d", h=2)),
                     reads=[ps], writes=[V])
    ar.reset(m1)
    numT = [ar.alloc((S,), F32, f"numT{i}") for i in range(2)]
    oTs = [ar.alloc((S,), BF16, f"doTs{i}") for i in range(2)]
    Es = Rot([ar.alloc((512,), BF16, f"E{i}") for i in range(3)])
    sel = ar.alloc((64,), F32, "sel64")
    P.op("pool", lambda e: e.memset(sel[:], 0.0), writes=[sel])
    P.op("pool", lambda e: e.memset(sel[64:65, :], 1.0), writes=[sel])
    sc = 64.0 ** -0.5
    for hl in range(2):
        rows = slice(hl * 64, hl * 64 + 64)
        for p, dd in enumerate(DIL):
            V = Vp[p]
            tpc = NTT // dd
            nb = min(4, tpc)
            for r in range(dd):
                for a0 in range(0, tpc, nb):
                    acc = cx.pacc.get()
                    first = True
                    for ak in range(max(a0 - 1, 0), a0 + nb):
                        qa_lo = max(ak, a0)
                        qa_hi = min(ak + 1, a0 + nb - 1)
                        ncol = (qa_hi - qa_lo + 1) * 128
                        qst = dd * 128 * qa_lo + r
                        kst = dd * 128 * ak + r
                        Sb = cx.pS.get()
                        mm(P, Sb[:, 0:ncol], kT[rows, kst:kst + dd * 127 + 1:dd], qT[rows, qst:qst + dd * (ncol - 1) + 1:dd],
                           True, True, [kT, qT], [Sb])
                        E = Es.get()
                        actf(P, E[:, 0:ncol], Sb[:, 0:ncol], AF.Exp, [Sb], [E], scale=sc)
                        for qa in range(qa_lo, qa_hi + 1):
                            ec = (qa - qa_lo) * 128
                            msk = cx.TRI if qa == ak else cx.ATRI_GE
                            tt(P, "pool", E[:, ec:ec + 128], E[:, ec:ec + 128], msk[:], ALU.mult, [E, msk], [E])
                        oc = (qa_lo - a0) * 128
                        mm(P, acc[0:65, oc:oc + ncol], V[:, r * tpc + ak, hl, :], E[:, 0:ncol], first, ak == a0 + nb - 1, [V, E], [acc], skip=True)
                        first = False
                    st = dd * 128 * a0 + r
                    dst = numT[hl][0:65, st:st + dd * (nb * 128 - 1) + 1:dd]
                    if p == 0:
                        P.op("dve", lambda e, dst=dst, acc=acc, nb=nb: e.tensor_copy(out=dst, in_=acc[0:65, 0:nb * 128]), reads=[acc], writes=[numT[hl]])
                    else:
                        tt(P, "dve", dst, acc[0:65, 0:nb * 128], dst, ALU.add, [acc, numT[hl]], [numT[hl]])
        nt = numT[hl]
        for tb in range(8):
            sl = slice(tb * 512, (tb + 1) * 512)
            P.op("dve", lambda e, nt=nt, sl=sl: e.reciprocal(out=nt[64:65, sl], in_=nt[64:65, sl]), reads=[nt], writes=[nt])
            ps = cx.pp.get()
            mm(P, ps[0:64, :], sel[0:65, 0:64], nt[0:65, sl], True, True, [sel, nt], [ps])
            tt(P, "dve", oTs[hl][0:64, sl], nt[0:64, sl], ps[0:64, :], ALU.mult, [nt, ps], [oTs[hl]])
        P.dma("sp", oT[384 + hl * 64:384 + hl * 64 + 64, :], oTs[hl][0:64, :], reads=[oTs[hl]], writes=[oT])
    ar.reset(m0)


def mixer_A(P, cx, wa, wck, wcv, posT, c2s, force, rope64, pswap64, oT, hp):
    ar = cx.ar
    m0 = ar.mark()
    QR = [ar.alloc((S,), BF16, f"aQR{i}") for i in range(2)]
    QROT = ar.alloc((S,), BF16, "aQROT")
    KVC = ar.alloc((S + 32,), BF16, "aKVC")
    KS2 = ar.alloc((S,), BF16, "aKS2")
    KW2 = ar.alloc((S,), BF16, "aKW2")
    VSW = ar.alloc((NTT, 2, 65), BF16, "aVSW")
    G = ar.alloc((NTT, 12), F32, "aG")
    m1 = ar.mark()
    W = load_w(P, cx, wa, 780, "WA")
    CC = ar.alloc((S,), F32, "CC")
    SS = ar.alloc((S,), F32, "SS")
    P.dma("sp", CC[:], rope64[0], writes=[CC])
    P.dma("sp", SS[:], rope64[1], writes=[SS])
    psw = ar.alloc((128,), BF16, "psw")
    P.dma("pool", psw[:], pswap64[:, :], writes=[psw])
    cx.ropeq = Rot([ar.alloc((512,), BF16, f"rq{i}") for i in range(2)])
    cx.ropet = Rot([ar.alloc((512,), F32, f"rt{i}") for i in range(3)])

    def plain(dst):
        def f(ps, tb):
            P.op("act", lambda e: e.copy(out=dst[:, tb * 512:(tb + 1) * 512], in_=ps[:, :]), reads=[ps], writes=[dst])
        return f
    for t in range(2):
        if t == hp:
            proj_fm(P, cx, W, t * 128, 128, lambda ps, tb, t=t: rope_evac(P, cx, ps, tb, 128, QROT, psw, CC, SS, raw_dst=QR[t]))
        else:
            proj_fm(P, cx, W, t * 128, 128, plain(QR[t]))
    P.op("pool", lambda e: e.memset(KVC[:, S:S + 32], 0.0), writes=[KVC])
    proj_fm(P, cx, W, 256, 128, plain(KVC))
    proj_fm(P, cx, W, 384, 128, lambda ps, tb: rope_evac(P, cx, ps, tb, 128, KS2, psw, CC, SS))
    proj_fm(P, cx, W, 512, 128, lambda ps, tb: rope_evac(P, cx, ps, tb, 128, KW2, psw, CC, SS))
    P.op("pool", lambda e: e.memset(VSW[:], 1.0), writes=[VSW])
    for j in range(NTT):
        ps = cx.pp.get()
        proj_tm(P, cx, W, 640, 140, j, ps)
        P.op("act", lambda e, j=j, ps=ps: e.copy(out=VSW[:, j, :, 0:64], in_=ps[:, 0:128].rearrange("p (h d) -> p h d", h=2)),
             reads=[ps], writes=[VSW])
        actf(P, G[:, j, :], ps[:, 128:140], AF.Exp, [ps], [G], scale=-1.0)
    ts(P, "dve", G[:], G[:], 1.0, None, ALU.add, None, [G], [G])
    P.op("dve", lambda e: e.reciprocal(out=G[:], in_=G[:]), reads=[G], writes=[G])
    ar.reset(m1)
    if STOP == 30:
        ar.reset(m0)
        return
    OACC = ar.alloc((NTT, 2, 64), F32, "aOACC")
    WC = ar.alloc((32, 128), BF16, "aWC")
    kcT2 = ar.alloc((256,), BF16, "akcT2")
    VC = ar.alloc((2, 129), BF16, "aVC")
    SELB = ar.alloc((S,), BF16, "aSELB")
    BSEL = ar.alloc((S,), BF16, "aBSEL")
    FORCE = ar.alloc((NTT, 64), F32, "aFORCE")
    oTs = ar.alloc((S,), BF16, "aoTs")
    Es = Rot([ar.alloc((512,), BF16, f"E{i}") for i in range(3)])
    P.op("pool", lambda e: e.memset(WC[:], 0.0), writes=[WC])
    P.dma("pool", WC[0:64, :, 0:64], wck[:, :, :], reads=[WC], writes=[WC])
    P.dma("pool", WC[0:64, :, 64:128], wck[:, :, :], reads=[WC], writes=[WC])
    P.dma("pool", WC[64:128, :, 0:64], wcv[:, :, :], reads=[WC], writes=[WC])
    P.dma("sp", FORCE[:], force[:, :, :], writes=[FORCE])
    P.op("pool", lambda e: e.memset(VC[:], 1.0), writes=[VC])
    P.dma("pool", VC[:, 0, 65:129], c2s[0:128, :], reads=[VC], writes=[VC])
    P.dma("pool", VC[:, 1, 65:129], c2s[128:256, :], reads=[VC], writes=[VC])
    P.op("pool", lambda e: e.memset(BSEL[0:64, :], 1.0), writes=[BSEL])
    P.op("pool", lambda e: e.affine_select(out=BSEL[0:64, :], in_=BSEL[0:64, :], pattern=[[1, S]], compare_op=ALU.is_ge, fill=0.0,
                                           base=0, channel_multiplier=-64), reads=[BSEL], writes=[BSEL])
    P.op("pool", lambda e: e.affine_select(out=BSEL[0:64, :], in_=BSEL[0:64, :], pattern=[[-1, S]], compare_op=ALU.is_ge, fill=0.0,
                                           base=63, channel_multiplier=64), reads=[BSEL], writes=[BSEL])
    P.dma("sp", BSEL[64:128, :], BSEL[0:64, :], reads=[BSEL], writes=[BSEL])
    KVA = ar.alloc((S + 32,), BF16, "aKVA")
    KVB = ar.alloc((S + 32,), BF16, "aKVB")
    nper = (S + 32) // 16
    pf = ar.alloc((32,), F32, "aposf")
    P.dma("sp", pf[:], posT[:, :], writes=[pf])
    for (dst, lo) in ((KVA, 0), (KVB, 16)):
        tt(P, "dve", dst[:].rearrange("p (a b) -> p a b", b=16), KVC[:].rearrange("p (a b) -> p a b", b=16),
           pf[:, lo:lo + 16].unsqueeze(1).to_broadcast([128, nper, 16]), ALU.add, [KVC, pf], [dst])
    pk = cx.pp.get()
    for l in range(32):
        src = KVA if l < 16 else KVB
        mm(P, pk[:, 0:256], WC[0:64, l, :], src[0:64, l:l + 16 * 255 + 1:16], l == 0, l == 31, [WC, src], [pk])
    P.op("dve", lambda e: e.tensor_copy(out=kcT2[:], in_=pk[:, 0:256]), reads=[pk], writes=[kcT2])
    for nt in range(2):
        pv = cx.pp.get()
        for l in range(32):
            src = KVA if l < 16 else KVB
            st = l + 16 * 128 * nt
            mm(P, pv[:, 0:64], src[64:128, st:st + 16 * 127 + 1:16], WC[64:128, l, 0:64], l == 0, l == 31, [WC, src], [pv])
        P.op("act", lambda e, nt=nt, pv=pv: e.copy(out=VC[:, nt, 0:64], in_=pv[:, 0:64]), reads=[pv], writes=[VC])
    if STOP == 32:
        ar.reset(m0)
        return
    sc = 0.125
    pS5 = Rot(cx.pS.b + cx.pp.b)
    sm = Rot([ar.alloc((16,), F32, f"asm{i}") for i in range(2)])
    imp_ = Rot([ar.alloc((64,), F32, f"aimp{i}") for i in range(2)])
    scr_ = Rot([ar.alloc((64,), F32, f"ascr{i}") for i in range(2)])
    sc2_ = Rot([ar.alloc((64,), F32, f"asc2{i}") for i in range(2)])
    val_ = Rot([ar.alloc((64,), F32, f"aval{i}") for i in range(2)])
    m8_ = Rot([ar.alloc((16,), F32, f"am8{i}") for i in range(2)])
    sb_ = Rot([ar.alloc((128,), BF16, f"asb{i}") for i in range(2)])
    for qt in range(NTT):
        nts = [0] if qt < 16 else [0, 1]
        bx, by = cx.pacc.get(), cx.pacc.get()
        bk = [bx, by]
        for ni, nt in enumerate(nts):
            Sbs = [pS5.get(), pS5.get()]
            E = Es.get()
            for par in range(2):
                rows = slice(par * 64, par * 64 + 64)
                for pair in range(2):
                    mm(P, Sbs[par][:, pair * 128:(pair + 1) * 128], kcT2[rows, nt * 128:(nt + 1) * 128], QR[pair][rows, qt * 128:(qt + 1) * 128],
                       True, True, [kcT2, QR[pair]], [Sbs[par]], skip=True)
                actf(P, E[:, par * 256:(par + 1) * 256], Sbs[par][:, 0:256], AF.Exp, [Sbs[par]], [E], scale=sc)
            base = 128 * qt - 31 - 16 * 128 * nt
            P.op("pool", lambda e, E=E, base=base: e.affine_select(out=E[:, :], in_=E[:, :], pattern=[[0, 4], [1, 128]], compare_op=ALU.is_ge,
                                                                   fill=0.0, base=base, channel_multiplier=-16), reads=[E], writes=[E])
            for h in range(4):
                b_ = bk[h // 2]
                o0 = (h % 2) * 129
                ec = (h % 2) * 256 + (h // 2) * 128
                mm(P, b_[:, o0:o0 + 129], E[:, ec:ec + 128], VC[:, nt, :], ni == 0 and h % 2 == 0, ni == len(nts) - 1,
                   [E, VC], [b_], skip=True)
        s_ = sm.get()
        for t in range(2):
            bv = bk[t][:, 0:258].rearrange("p (a b) -> p a b", a=2)
            ts(P, "dve", s_[:, 2 * t:2 * t + 2], bv[:, :, 64], 1.0e-30, None, ALU.max, None, [bk[t]], [s_])
        P.op("dve", lambda e, s_=s_: e.reciprocal(out=s_[:, 0:4], in_=s_[:, 0:4]), reads=[s_], writes=[s_])
        imp = imp_.get()
        for h in range(4):
            src = bk[h // 2][:, (h % 2) * 129 + 65:(h % 2) * 129 + 129]
            if h == 0:
                ts(P, "dve", imp[:], src, s_[:, 0:1], None, ALU.mult, None, [bk[0], s_], [imp])
            else:
                stt(P, imp[:], src, s_[:, h:h + 1], imp[:], ALU.mult, ALU.add, [bk[h // 2], s_, imp], [imp])
        for hl in range(2):
            h = 2 * hp + hl
            tt(P, "dve", s_[:, 4 + hl:5 + hl], s_[:, h:h + 1], G[:, qt, h:h + 1], ALU.mult, [s_, G], [s_])
            ts(P, "dve", OACC[:, qt, hl, :], bk[h // 2][:, (h % 2) * 129:(h % 2) * 129 + 64], s_[:, 4 + hl:5 + hl], None, ALU.mult, None,
               [bk[h // 2], s_], [OACC])
        scr, sc2, val, m8, sb = scr_.get(), sc2_.get(), val_.get(), m8_.get(), sb_.get()
        fr = FORCE[:, qt, :]
        ts(P, "dve", val[:], fr, 0.0, None, ALU.is_equal, None, [FORCE], [val])
        tt(P, "dve", scr[:], imp[:], val[:], ALU.mult, [imp, val], [scr])
        tt(P, "dve", scr[:], scr[:], fr, ALU.add, [scr, FORCE], [scr])
        P.op("dve", lambda e, m8=m8, scr=scr: e.max(out=m8[:, 0:8], in_=scr[:]), reads=[scr], writes=[m8])
        P.op("dve", lambda e, m8=m8, scr=scr, sc2=sc2: e.match_replace(out=sc2[:], in_to_replace=m8[:, 0:8], in_values=scr[:], imm_value=-3.0e38),
             reads=[scr, m8], writes=[sc2])
        P.op("dve", lambda e, m8=m8, sc2=sc2: e.max(out=m8[:, 8:16], in_=sc2[:]), reads=[sc2], writes=[m8])
        ts(P, "dve", sc2[:], scr[:], m8[:, 15:16], None, ALU.is_ge, None, [scr, m8], [sc2])
        ts(P, "dve", val[:], fr, 0.0, None, ALU.is_ge, None, [FORCE], [val])
        tt(P, "dve", sc2[:], sc2[:], val[:], ALU.mult, [sc2, val], [sc2])
        ts(P, "dve", sb[:, 0:64], sc2[:], -1.0, 30000.0, ALU.add, ALU.mult, [sc2], [sb])
        ts(P, "dve", sb[:, 64:128], sc2[:], -1.0, 30000.0, ALU.add, ALU.mult, [sc2], [sb])
        pt = cx.ptp.get()
        P.op("pe", lambda e, pt=pt, sb=sb: e.transpose(out=pt[:, 0:128], in_=sb[:], identity=cx.ident[:]), reads=[sb, cx.ident], writes=[pt])
        P.op("act", lambda e, pt=pt, qt=qt: e.copy(out=SELB[:, qt * 128:(qt + 1) * 128], in_=pt[:, 0:128]), reads=[pt], writes=[SELB])
    if STOP == 31:
        P.dma("sp", oT[0:64, :], SELB[0:64, :], reads=[SELB], writes=[oT])
        ar.reset(m0)
        return
    rr = Rot([ar.alloc((8,), F32, f"arr{i}") for i in range(2)])

    def branch(br, K2, vidx, use_sel):
        for QB in range(NTT // 2):
            acc = cx.pacc.get()
            first = True
            kt_lo = 0 if use_sel else max(0, 2 * QB - 4)
            for kt in range(kt_lo, 2 * QB + 2):
                E = Es.get()
                for hl in range(2):
                    Sb = pS5.get()
                    rows = slice(hl * 64, hl * 64 + 64)
                    mm(P, Sb[:, 0:256], K2[rows, kt * 128:(kt + 1) * 128], QROT[rows, QB * 256:(QB + 1) * 256],
                       True, not use_sel, [K2, QROT], [Sb], skip=True)
                    if use_sel:
                        mm(P, Sb[:, 0:256], BSEL[rows, kt * 128:(kt + 1) * 128], SELB[rows, QB * 256:(QB + 1) * 256],
                           False, True, [BSEL, SELB], [Sb], skip=True)
                    actf(P, E[:, hl * 256:(hl + 1) * 256], Sb[:, 0:256], AF.Exp, [Sb], [E], scale=sc)
                for ql in range(2):
                    qa = 2 * QB + ql
                    dlt = qa - kt
                    if dlt < 0 or (not use_sel and dlt > 4):
                        continue
                    msk = None
                    if dlt == 0:
                        msk = cx.TRI
                    elif not use_sel and dlt == 4:
                        msk = cx.ATRI_S
                    for hl in range(2):
                        ec = hl * 256 + ql * 128
                        if msk is not None:
                            tt(P, "pool", E[:, ec:ec + 128], E[:, ec:ec + 128], msk[:], ALU.mult, [E, msk], [E])
                        oc = (hl * 2 + ql) * 65
                        mm(P, acc[:, oc:oc + 65], E[:, ec:ec + 128], VSW[:, kt, vidx, :], first, kt == qa, [E, VSW], [acc], skip=True)
                        first = False
            r_ = rr.get()
            accv = acc[:, 0:260].rearrange("p (a b) -> p a b", a=4)
            P.op("dve", lambda e, r_=r_, accv=accv: e.reciprocal(out=r_[:, 0:4], in_=accv[:, :, 64]), reads=[acc], writes=[r_])
            for hl in range(2):
                h = 2 * hp + hl
                for ql in range(2):
                    qa = 2 * QB + ql
                    i4 = hl * 2 + ql
                    tt(P, "dve", r_[:, 4 + i4:5 + i4], r_[:, i4:i4 + 1], G[:, qa, br * 4 + h:br * 4 + h + 1], ALU.mult, [r_, G], [r_])
                    stt(P, OACC[:, qa, hl, :], acc[:, i4 * 65:i4 * 65 + 64], r_[:, 4 + i4:5 + i4], OACC[:, qa, hl, :], ALU.mult, ALU.add,
                        [acc, r_, OACC], [OACC])
    if STOP not in (37, 38):
        branch(1, KS2, 0, True)
    if STOP == 36:
        ar.reset(m0)
        return
    if STOP != 38:
        branch(2, KW2, 1, False)
    if STOP == 37:
        ar.reset(m0)
        return
    ob_ = Rot([ar.alloc((128,), BF16, f"aob{i}") for i in range(2)])
    for j in range(NTT):
        ob = ob_.get()
        P.op("dve", lambda e, ob=ob, j=j: e.tensor_copy(out=ob[:], in_=OACC[:, j, :, :].rearrange("p a b -> p (a b)")), reads=[OACC], writes=[ob])
        pt = cx.ptp.get()
        P.op("pe", lambda e, pt=pt, ob=ob: e.transpose(out=pt[:, 0:128], in_=ob[:], identity=cx.ident[:]), reads=[ob, cx.ident], writes=[pt])
        P.op("act", lambda e, pt=pt, j=j: e.copy(out=oTs[:, j * 128:(j + 1) * 128], in_=pt[:, 0:128]), reads=[pt], writes=[oTs])
    P.dma("sp", oT[0:128, :], oTs[:], reads=[oTs], writes=[oT])
    ar.reset(m0)


def mixer_C(P, cx, wc, convw, convb, wq, wk, gbias, mnorm, oT):
    ar = cx.ar
    m0 = ar.mark()
    ucT = ar.alloc((S,), BF16, "cucT")
    QT = ar.alloc((S,), BF16, "cQT")
    QE = ar.alloc((S,), BF16, "cQE")
    QO = ar.alloc((S,), BF16, "cQO")
    KT = ar.alloc((S,), BF16, "cKT")
    KTM = ar.alloc((NTT, 128), BF16, "cKTM")
    VTM = ar.alloc((NTT, 128), BF16, "cVTM")
    SO = ar.alloc((NTT, 128), F32, "cSO")
    IFG = ar.alloc((NTT, 4), F32, "cIF")
    oTs = ar.alloc((S,), BF16, "coTs")
    WQ = ar.alloc((128,), BF16, "cWQ")
    WK = ar.alloc((128,), BF16, "cWK")
    m1 = ar.mark()
    W = load_w(P, cx, wc, 388, "WC")
    UP = ar.alloc((S + 8,), F32, "cUP")
    UC = ar.alloc((S,), F32, "cUC")
    cw = ar.alloc((8,), F32, "ccw")
    P.dma("sp", cw[:, 0:4], convw[:, :], writes=[cw])
    P.dma("sp", cw[:, 4:5], convb[:, :], writes=[cw])
    for Wt, src in ((WQ, wq), (WK, wk)):
        P.op("pool", lambda e, Wt=Wt: e.memset(Wt[:], 0.0), writes=[Wt])
        for hl in range(2):
            P.dma("pool", Wt[hl * 64:(hl + 1) * 64, hl * 64:(hl + 1) * 64], src[hl], reads=[Wt], writes=[Wt])
    EM = ar.alloc((512,), BF16, "cEM")
    OM = ar.alloc((512,), BF16, "cOM")
    P.op("pool", lambda e: e.memset(EM[:], 1.0), writes=[EM])
    P.op("pool", lambda e: e.memset(EM[:].rearrange("p (a g c) -> p a g c", g=2, c=64)[:, :, 1, :], 0.0), reads=[EM], writes=[EM])
    P.op("pool", lambda e: e.memset(OM[:], 1.0), writes=[OM])
    P.op("pool", lambda e: e.memset(OM[:].rearrange("p (a g c) -> p a g c", g=2, c=64)[:, :, 0, :], 0.0), reads=[OM], writes=[OM])
    P.op("pool", lambda e: e.memset(UP[:, 0:8], 0.0), writes=[UP])

    def evu(ps, tb):
        P.op("act", lambda e: e.copy(out=UP[:, 3 + tb * 512:3 + (tb + 1) * 512], in_=ps[:, :]), reads=[ps], writes=[UP])
    proj_fm(P, cx, W, 0, 128, evu)
    ts(P, "dve", UC[:], UP[:, 3:3 + S], cw[:, 3:4], cw[:, 4:5], ALU.mult, ALU.add, [UP, cw], [UC])
    for jj in range(3):
        stt(P, UC[:], UP[:, jj:jj + S], cw[:, jj:jj + 1], UC[:], ALU.mult, ALU.add, [UP, cw, UC], [UC])
    actf(P, UP[:, 0:S], UC[:], AF.Exp, [UC], [UP], scale=-1.0)
    ts(P, "dve", UP[:, 0:S], UP[:, 0:S], 1.0, None, ALU.add, None, [UP], [UP])
    P.op("dve", lambda e: e.reciprocal(out=UP[:, 0:S], in_=UP[:, 0:S]), reads=[UP], writes=[UP])
    tt(P, "dve", ucT[:], UC[:], UP[:, 0:S], ALU.mult, [UC, UP], [ucT])
    for tb in range(8):
        sl = slice(tb * 512, (tb + 1) * 512)
        pq = cx.pp.get()
        mm(P, pq[:, :], WQ[:, :], ucT[:, sl], True, True, [WQ, ucT], [pq])
        P.op("act", lambda e, pq=pq, sl=sl: e.copy(out=QT[:, sl], in_=pq[:, :]), reads=[pq], writes=[QT])
        tt(P, "dve", QE[:, sl], pq[:, :], EM[:], ALU.mult, [pq, EM], [QE])
        tt(P, "dve", QO[:, sl], pq[:, :], OM[:], ALU.mult, [pq, OM], [QO])
        pk = cx.pp.get()
        mm(P, pk[:, :], WK[:, :], ucT[:, sl], True, True, [WK, ucT], [pk])
        P.op("act", lambda e, pk=pk, sl=sl: e.mul(out=KT[:, sl], in_=pk[:, :], mul=0.125), reads=[pk], writes=[KT])
    for j in range(NTT):
        ps = cx.pp.get()
        proj_tm(P, cx, W, 128, 260, j, ps)
        P.op("act", lambda e, j=j, ps=ps: e.copy(out=VTM[:, j, :], in_=ps[:, 0:128]), reads=[ps], writes=[VTM])
        actf(P, SO[:, j, :], ps[:, 128:256], AF.Exp, [ps], [SO], scale=-1.0)
        P.op("act", lambda e, j=j, ps=ps: e.copy(out=IFG[:, j, :], in_=ps[:, 256:260]), reads=[ps], writes=[IFG])
        pk = cx.pp.get()
        mm(P, pk[:, 0:128], ucT[:, j * 128:(j + 1) * 128], WK[:, :], True, True, [WK, ucT], [pk])
        P.op("act", lambda e, j=j, pk=pk: e.mul(out=KTM[:, j, :], in_=pk[:, 0:128], mul=0.125), reads=[pk], writes=[KTM])
    ts(P, "dve", SO[:], SO[:], 1.0, None, ALU.add, None, [SO], [SO])
    P.op("dve", lambda e: e.reciprocal(out=SO[:], in_=SO[:]), reads=[SO], writes=[SO])
    ar.reset(m1)
    GB = ar.alloc((4,), F32, "cGB")
    P.dma("sp", GB[:], gbias[0:1, :].partition_broadcast(128), writes=[GB])
    MN = ar.alloc((128,), F32, "cMN")
    P.dma("sp", MN[:], mnorm[0:1, :].partition_broadcast(128), writes=[MN])
    IG = ar.alloc((NTT, 2), F32, "cIG")
    LF = ar.alloc((NTT, 2), F32, "cLF")
    Bc = ar.alloc((NTT, 2), F32, "cB")
    Wg = ar.alloc((NTT, 2), F32, "cWg")
    EB = ar.alloc((NTT, 2), F32, "cEB")
    EA = ar.alloc((2, NTT, 2), F32, "cEA")
    EAh = ar.alloc((2, NTT), F32, "cEAh")
    BDU = ar.alloc((128,), F32, "cBDU")
    OG = [ar.alloc((128,), F32, f"cOG{g}") for g in range(2)]
    BDT = ar.alloc((128,), BF16, "cBDT")
    tt(P, "dve", IG[:], IFG[:, :, 0:2], GB[:, 0:2].unsqueeze(1).to_broadcast([128, NTT, 2]), ALU.add, [IFG, GB], [IG])
    tt(P, "dve", LF[:], IFG[:, :, 2:4], GB[:, 2:4].unsqueeze(1).to_broadcast([128, NTT, 2]), ALU.add, [IFG, GB], [LF])
    actf(P, LF[:], LF[:], AF.Exp, [LF], [LF], scale=-1.0)
    ts(P, "dve", LF[:], LF[:], 1.0, None, ALU.add, None, [LF], [LF])
    actf(P, LF[:], LF[:], AF.Ln, [LF], [LF])
    ts(P, "dve", LF[:], LF[:], -1.0, None, ALU.mult, None, [LF], [LF])
    P.op("pool", lambda e: e.memset(BDU[:], 1.0), writes=[BDU])
    P.op("pool", lambda e: e.affine_select(out=BDU[:], in_=BDU[:], pattern=[[1, 128]], compare_op=ALU.is_ge, fill=0.0, base=0,
                                           channel_multiplier=-1), reads=[BDU], writes=[BDU])
    P.op("pool", lambda e: e.memset(BDU[0:64, 64:128], 0.0), reads=[BDU], writes=[BDU])
    P.op("pool", lambda e: e.tensor_copy(out=BDT[:], in_=BDU[:]), reads=[BDU], writes=[BDT])
    for g in range(2):
        P.op("pool", lambda e, g=g: e.memset(OG[g][:], 0.0), writes=[OG[g]])
        P.op("pool", lambda e, g=g: e.memset(OG[g][g * 64:(g + 1) * 64, :], 1.0), reads=[OG[g]], writes=[OG[g]])
    LFv = LF[:].rearrange("p a b -> p (a b)")
    pb = cx.pp.get()
    mm(P, pb[:, 0:64], BDU[:], LFv, True, True, [BDU, LF], [pb])
    P.op("dve", lambda e: e.tensor_copy(out=Bc[:].rearrange("p a b -> p (a b)"), in_=pb[:, 0:64]), reads=[pb], writes=[Bc])
    pa = cx.pp.get()
    for g in range(2):
        mm(P, pa[:, g * 64:(g + 1) * 64], OG[g][:], LFv, True, True, [OG[g], LF], [pa], skip=True)
    actf(P, EA[:].rearrange("p g a b -> p (g a b)"), pa[:, 0:128], AF.Exp, [pa], [EA])
    for hl in range(2):
        rows = slice(hl * 64, hl * 64 + 64)
        P.op("dve", lambda e, rows=rows, hl=hl: e.tensor_copy(out=EAh[rows, :, :], in_=EA[rows, :, :, hl]), reads=[EA], writes=[EAh])
    tt(P, "dve", Wg[:], IG[:], Bc[:], ALU.subtract, [IG, Bc], [Wg])
    actf(P, Wg[:], Wg[:], AF.Exp, [Wg], [Wg])
    actf(P, EB[:], Bc[:], AF.Exp, [Bc], [EB])
    pGT = Rot(cx.pS.b[0:2])
    pX = cx.pS.b[2]
    pY = cx.pacc.b
    pU = cx.pp.b
    Z = ar.alloc((65,), F32, "cZ")
    Sb0 = Rot([ar.alloc((65,), BF16, f"cSb0{i}") for i in range(2)])
    Sb1 = Rot([ar.alloc((65,), BF16, f"cSb1{i}") for i in range(2)])
    rv_ = Rot([ar.alloc((2, 65), BF16, f"crv{i}") for i in range(2)])
    AT_ = Rot([ar.alloc((128,), BF16, f"cAT{i}") for i in range(3)])
    XS_ = Rot([ar.alloc((130,), F32, f"cXS{i}") for i in range(2)])
    TOT_ = Rot([ar.alloc((65,), F32, f"cTOT{i}") for i in range(2)])
    HH_ = Rot([ar.alloc((128,), F32, f"cHH{i}") for i in range(2)])
    SQ_ = Rot([ar.alloc((128,), F32, f"cSQ{i}") for i in range(2)])
    sm_ = Rot([ar.alloc((8,), F32, f"csm{i}") for i in range(2)])
    hb_ = Rot([ar.alloc((128,), BF16, f"chb{i}") for i in range(2)])
    for j in range(NTT):
        tsl = slice(j * 128, (j + 1) * 128)
        rv = rv_.get()
        for hl in range(2):
            ts(P, "dve", rv[:, hl, 0:64], VTM[:, j, hl * 64:(hl + 1) * 64], Wg[:, j, hl:hl + 1], None, ALU.mult, None, [VTM, Wg], [rv])
        P.op("dve", lambda e, rv=rv, j=j: e.tensor_copy(out=rv[:, :, 64], in_=Wg[:, j, :]), reads=[Wg], writes=[rv])
        for g in range(2):
            for hl in range(2):
                mm(P, pU[g][hl * 64:(hl + 1) * 64, 0:65], KTM[g * 64:(g + 1) * 64, j, hl * 64:(hl + 1) * 64], rv[g * 64:(g + 1) * 64, hl, :],
                   True, True, [KTM, rv], [pU[g]], skip=True)
        s0, s1 = Sb0.get(), Sb1.get()
        if j == 0:
            P.op("pool", lambda e, s0=s0: e.memset(s0[:], 0.0), writes=[s0])
            P.op("dve", lambda e: e.tensor_copy(out=Z[:], in_=pU[0][:, 0:65]), reads=[pU[0]], writes=[Z])
        else:
            ts(P, "dve", s0[:], Z[:], EAh[:, 1, j - 1:j], None, ALU.mult, None, [Z, EAh], [s0])
            stt(P, Z[:], Z[:], EAh[:, 1, j - 1:j], pU[0][:, 0:65], ALU.mult, ALU.add, [Z, EAh, pU[0]], [Z])
        ts(P, "dve", s1[:], Z[:], EAh[:, 0, j:j + 1], None, ALU.mult, None, [Z, EAh], [s1])
        stt(P, Z[:], Z[:], EAh[:, 0, j:j + 1], pU[1][:, 0:65], ALU.mult, ALU.add, [Z, EAh, pU[1]], [Z])
        XS = XS_.get()
        for hl in range(2):
            rows = slice(hl * 64, hl * 64 + 64)
            gt = pGT.get()
            mm(P, gt[:, 0:128], KT[rows, tsl], QT[rows, tsl], True, True, [KT, QT], [gt])
            AT = AT_.get()
            tt(P, "dve", AT[:], gt[:, 0:128], BDT[:], ALU.mult, [gt, BDT], [AT])
            mm(P, pX[:, hl * 65:(hl + 1) * 65], AT[:], rv[:, hl, :], hl == 0, True, [AT, rv], [pX], skip=True)
            mm(P, pY[hl][:, 0:65], QE[rows, tsl], s0[rows, :], True, False, [QE, s0], [pY[hl]], skip=True)
            mm(P, pY[hl][:, 0:65], QO[rows, tsl], s1[rows, :], False, True, [QO, s1], [pY[hl]], skip=True)
        P.op("act", lambda e, XS=XS: e.copy(out=XS[:], in_=pX[:, 0:130]), reads=[pX], writes=[XS])
        HH, SQ, sm, hb = HH_.get(), SQ_.get(), sm_.get(), hb_.get()
        for hl in range(2):
            TOT = TOT_.get()
            tt(P, "dve", TOT[:], pY[hl][:, 0:65], XS[:, hl * 65:(hl + 1) * 65], ALU.add, [pY[hl], XS], [TOT])
            tt(P, "dve", sm[:, hl:hl + 1], TOT[:, 64:65], EB[:, j, hl:hl + 1], ALU.mult, [TOT, EB], [sm])
            ts(P, "dve", sm[:, 6 + hl:7 + hl], sm[:, hl:hl + 1], -1.0, None, ALU.mult, None, [sm], [sm])
            tt(P, "dve", sm[:, hl:hl + 1], sm[:, hl:hl + 1], sm[:, 6 + hl:7 + hl], ALU.max, [sm], [sm])
            ts(P, "dve", sm[:, hl:hl + 1], sm[:, hl:hl + 1], 1.0, None, ALU.max, None, [sm], [sm])
            P.op("dve", lambda e, sm=sm, hl=hl: e.reciprocal(out=sm[:, hl:hl + 1], in_=sm[:, hl:hl + 1]), reads=[sm], writes=[sm])
            tt(P, "dve", sm[:, 2 + hl:3 + hl], sm[:, hl:hl + 1], EB[:, j, hl:hl + 1], ALU.mult, [sm, EB], [sm])
            ts(P, "dve", HH[:, hl * 64:(hl + 1) * 64], TOT[:, 0:64], sm[:, 2 + hl:3 + hl], None, ALU.mult, None, [TOT, sm], [HH])
        tt(P, "dve", SQ[:], HH[:], HH[:], ALU.mult, [HH], [SQ])
        P.op("dve", lambda e, sm=sm, SQ=SQ: e.tensor_reduce(out=sm[:, 4:6], in_=SQ[:].rearrange("p (a b) -> p a b", a=2), axis=AX.X, op=ALU.add),
             reads=[SQ], writes=[sm])
        ts(P, "dve", sm[:, 4:6], sm[:, 4:6], 1.0 / 64, EPS, ALU.mult, ALU.add, [sm], [sm])
        actf(P, sm[:, 4:6], sm[:, 4:6], AF.Sqrt, [sm], [sm])
        P.op("dve", lambda e, sm=sm: e.reciprocal(out=sm[:, 4:6], in_=sm[:, 4:6]), reads=[sm], writes=[sm])
        for hl in range(2):
            stt(P, HH[:, hl * 64:(hl + 1) * 64], HH[:, hl * 64:(hl + 1) * 64], sm[:, 4 + hl:5 + hl], MN[:, hl * 64:(hl + 1) * 64], ALU.mult, ALU.mult,
                [HH, sm, MN], [HH])
        tt(P, "dve", hb[:], HH[:], SO[:, j, :], ALU.mult, [HH, SO], [hb])
        pt = cx.ptp.get()
        P.op("pe", lambda e, pt=pt, hb=hb: e.transpose(out=pt[:, 0:128], in_=hb[:], identity=cx.ident[:]), reads=[hb, cx.ident], writes=[pt])
        P.op("act", lambda e, pt=pt, tsl=tsl: e.copy(out=oTs[:, tsl], in_=pt[:, 0:128]), reads=[pt], writes=[oTs])
    P.dma("sp", oT[256:384, :], oTs[:], reads=[oTs], writes=[oT])
    ar.reset(m0)


def build_mixer(layer, mix="abcd"):
    P = Prog()
    cx = Cx()
    x = P.dram("x", [S, D], F32, "ExternalInput")
    gm = P.dram("gm", [1, D], F32, "ExternalInput")
    wb = P.dram("wb", [D, 384], F32, "ExternalInput")
    lamd = P.dram("lamd", [1, 128], F32, "ExternalInput")
    dnorm = P.dram("dnorm", [1, 64], F32, "ExternalInput")
    rope32 = P.dram("rope32", [2, 128, S], F32, "ExternalInput")
    pswap32 = P.dram("pswap32", [128, 128], F32, "ExternalInput")
    wd = P.dram("wd", [D, 384], F32, "ExternalInput")
    wa = P.dram("wa", [D, 780], F32, "ExternalInput")
    wc = P.dram("wc", [D, 388], F32, "ExternalInput")
    convw = P.dram("convw", [128, 4], F32, "ExternalInput")
    convb = P.dram("convb", [128, 1], F32, "ExternalInput")
    wq = P.dram("wq", [2, 64, 64], F32, "ExternalInput")
    wk = P.dram("wk", [2, 64, 64], F32, "ExternalInput")
    gbias = P.dram("gbias", [1, 4], F32, "ExternalInput")
    mnorm = P.dram("mnorm", [1, 128], F32, "ExternalInput")
    wck = P.dram("wck", [64, 32, 64], F32, "ExternalInput")
    wcv = P.dram("wcv", [64, 32, 64], F32, "ExternalInput")
    posT = P.dram("posT", [128, 32], F32, "ExternalInput")
    c2s = P.dram("c2s", [256, 64], F32, "ExternalInput")
    force = P.dram("force", [128, NTT, 64], F32, "ExternalInput")
    rope64 = P.dram("rope64", [2, 128, S], F32, "ExternalInput")
    pswap64 = P.dram("pswap64", [128, 128], F32, "ExternalInput")
    oT = P.dram("oT", [512, S], BF16, "ExternalOutput")
    cx.ar = Arena(P, 52000)
    banks = [P.ps([128, 512], F32, f"bank{i}") for i in range(7)] + [P.ps([128, 1024], BF16, "bank7")]
    cx.pS = Rot(banks[0:3])
    cx.pacc = Rot(banks[3:5])
    cx.pp = Rot(banks[5:7])
    cx.ptp = Rot(banks[7:8])
    cx.hT = cx.ar.alloc((8, S), BF16, "hT")
    build_consts(P, cx)
    stage0(P, cx, x, gm)
    if "a" in mix:
        mixer_A(P, cx, wa, wck, wcv, posT, c2s, force, rope64, pswap64, oT, 0)
    if "b" in mix:
        mixer_B(P, cx, wb, lamd, dnorm, rope32, pswap32, oT, layer)
    if "c" in mix:
        mixer_C(P, cx, wc, convw, convb, wq, wk, gbias, mnorm, oT)
    if "d" in mix:
        mixer_D(P, cx, wd, rope64, pswap64, oT)
    return P.finish()


def _rope_tab(dim, rows_per_block):
    inv = (1.0 / (np.float32(10000.0) ** (np.arange(0, dim, 2, dtype=np.float32) / np.float32(dim)))).astype(np.float32)
    ang = (np.arange(S, dtype=np.float32)[:, None] * inv[None, :]).astype(np.float32)
    c = np.cos(ang.astype(np.float64)).astype(np.float32).T
    s = np.sin(ang.astype(np.float64)).astype(np.float32).T
    half = dim // 2
    idx = np.arange(128) % half
    return np.ascontiguousarray(np.stack([c[idx], s[idx]], 0))


def _pswapT(dim):
    half = dim // 2
    Pm = np.zeros((128, 128), np.float32)
    for m_ in range(128):
        if (m_ % dim) < half:
            Pm[m_, m_ + half] = -1.0
        else:
            Pm[m_, m_ - half] = 1.0
    return np.ascontiguousarray(Pm.T)


def _cmp_to_slc():
    n_cmp, n_slc = 255, 64
    jj = np.arange(n_slc)[:, None, None]
    src = 4 * jj - np.arange(4)[None, :, None] - np.arange(2)[None, None, :]
    ok = (src >= 0) & (src < n_cmp)
    c = np.zeros((256, n_slc), np.float32)
    np.add.at(c, (np.where(ok, src, 0), np.broadcast_to(jj, src.shape)), ok.astype(np.float32))
    return c


def _nsa_force():
    t = np.arange(S)
    cur = (t // 64)[:, None]
    blk = np.arange(64)[None, :]
    forced = (blk == 0) | ((blk <= cur) & (blk > cur - 2))
    f = np.where(blk > cur, -1.0e6, np.where(forced, 1.0e6, 0.0)).astype(np.float32)
    return np.ascontiguousarray(f.reshape(NTT, 128, 64).transpose(1, 0, 2))


OFF = np.concatenate([[0], np.cumsum([256, 64, 64, 64, 64, 64, 64, 12, 256, 256, 256, 256, 256, 4, 4, 256, 256, 256, 256])]).astype(int)
(A_Q, A_KC, A_VC, A_KS, A_VS, A_KW, A_VW, A_G, B_Q, B_K, B_V, C_U, C_V, C_I, C_F, C_O, D_Q, D_K, D_V) = [int(o) for o in OFF[:-1]]


def mixer_inputs(inp, layer, xb, hp):
    w = inp["w_in"][layer]
    own = lambda off: np.arange(off + hp * 128, off + hp * 128 + 128)
    d = dict(x=np.ascontiguousarray(xb), gm=np.ascontiguousarray(inp["norm_mix"][layer][None]))
    d["wb"] = np.ascontiguousarray(w[:, np.concatenate([own(B_Q), own(B_K), own(B_V)])])
    d["lamd"] = np.ascontiguousarray(inp["diff_lambda"][layer].reshape(1, 128))
    d["dnorm"] = np.ascontiguousarray(inp["diff_norm"][layer][None])
    d["rope32"] = _rope_tab(32, 32)
    d["pswap32"] = _pswapT(32)
    oth = 1 - hp
    gcols = np.concatenate([[A_G + br * 4 + 2 * hp, A_G + br * 4 + 2 * hp + 1, A_G + br * 4 + 2 * oth, A_G + br * 4 + 2 * oth + 1] for br in range(3)])
    cols = np.concatenate([np.arange(A_Q + hp * 128, A_Q + hp * 128 + 128), np.arange(A_Q + oth * 128, A_Q + oth * 128 + 128), np.arange(A_KC, A_KC + 128), np.arange(A_KS, A_KS + 64), np.arange(A_KS, A_KS + 64),
                           np.arange(A_KW, A_KW + 64), np.arange(A_KW, A_KW + 64), np.arange(A_VS, A_VS + 64), np.arange(A_VW, A_VW + 64), gcols])
    d["wa"] = np.ascontiguousarray(w[:, cols])
    cw = inp["nsa_cmp_w"][layer]
    d["wck"] = np.ascontiguousarray(cw[0].reshape(32, 64, 64).transpose(1, 0, 2))
    d["wcv"] = np.ascontiguousarray(cw[1].reshape(32, 64, 64).transpose(1, 0, 2))
    cp = inp["nsa_cmp_pos"][layer]
    d["posT"] = np.ascontiguousarray(np.concatenate([cp[0].T, cp[1].T], 0))
    d["c2s"] = _cmp_to_slc()
    d["force"] = _nsa_force()
    d["wc"] = np.ascontiguousarray(w[:, np.concatenate([own(C_U), own(C_V), own(C_O), [C_I + 2 * hp, C_I + 2 * hp + 1, C_F + 2 * hp, C_F + 2 * hp + 1]])])
    d["convw"] = np.ascontiguousarray(inp["mlstm_conv_w"][layer][:, hp * 128:(hp + 1) * 128].T)
    d["convb"] = np.ascontiguousarray(inp["mlstm_conv_b"][layer][hp * 128:(hp + 1) * 128][:, None])
    d["wq"] = np.ascontiguousarray(inp["mlstm_wq"][layer][2 * hp:2 * hp + 2])
    d["wk"] = np.ascontiguousarray(inp["mlstm_wk"][layer][2 * hp:2 * hp + 2])
    gbl = inp["mlstm_gate_b"][layer]
    d["gbias"] = np.ascontiguousarray(np.concatenate([gbl[0, 2 * hp:2 * hp + 2], gbl[1, 2 * hp:2 * hp + 2]])[None])
    d["mnorm"] = np.ascontiguousarray(inp["mlstm_norm"][layer][hp * 128:(hp + 1) * 128][None])
    d["wd"] = np.ascontiguousarray(w[:, np.concatenate([own(D_Q), own(D_K), own(D_V)])])
    d["rope64"] = _rope_tab(64, 64)
    d["pswap64"] = _pswapT(64)
    return d


def kernel(**inputs):
    inp = {k: np.asarray(v) for k, v in inputs.items()}
    x = np.ascontiguousarray(inp["x"], dtype=np.float32)
    nb = x.shape[0]
    cores = list(range(8))
    for layer in range(2):
        ncm = build_mixer(layer)
        maps = [mixer_inputs(inp, layer, x[c // 2], c % 2) for c in cores]
        res = run_bass_kernel_spmd(ncm, maps, core_ids=cores)
        oTs = [np.asarray(r["oT"]) for r in res.results]
        ncf = build_ffn(layer == 1)
        maps = []
        for c in cores:
            b, half = c // 2, c % 2
            sl = slice(half * 2048, (half + 1) * 2048)
            oT = np.empty((1024, 2048), dtype=oTs[0].dtype)
            for m in range(4):
                for hp in range(2):
                    oT[m * 256 + hp * 128:m * 256 + hp * 128 + 128] = oTs[2 * b + hp][m * 128:(m + 1) * 128, sl]
            maps.append(dict(xin=np.ascontiguousarray(x[b, sl]), oT=oT,
                             w_out=inp["w_out"][layer], w_gate=inp["w_gate"][layer], w_up=inp["w_up"][layer], w_down=inp["w_down"][layer],
                             gf=np.ascontiguousarray(inp["norm_ffn"][layer][None]), gl=np.ascontiguousarray(inp["norm_final"][None])))
        res = run_bass_kernel_spmd(ncf, maps, core_ids=cores)
        xn = np.empty_like(x)
        for c in cores:
            b, half = c // 2, c % 2
            xn[b, half * 2048:(half + 1) * 2048] = np.asarray(res.results[c]["xout"])
        x = xn
    return x.astype(np.float32)
```
